# Optimizing a Trainium2 kernel written in Bass

```python
import jax
import jax.numpy as jnp
from jax import lax
import numpy as np

D_MODEL = 1024
BATCH = 8
SEQ = 4096
DEPTH = 1

CHUNK = 64
Q_BLOCK = 128
LN_EPS = 1e-5
FOX_HEADS = 8
FOX_HEAD_DIM = 64
FOX_WIDTH = FOX_HEADS * FOX_HEAD_DIM
GLA_HEADS = 4
GLA_KEY_DIM = 64
GLA_VALUE_DIM = 128
GLA_KEY_WIDTH = GLA_HEADS * GLA_KEY_DIM
GLA_WIDTH = GLA_HEADS * GLA_VALUE_DIM
GLA_GATE_RANK = 16
GLA_GATE_TEMP = 16.0
MIX_WIDTH = FOX_WIDTH + GLA_WIDTH
N_EXPERTS = 256
N_GROUPS = 8
TOPK_GROUPS = 4
TOP_K = 8
EXPERT_DIM = 256
SHARED_DIM = 256
ROUTED_SCALE = 2.5
EXPERT_ROWS = 64
DEEPNORM_ALPHA = (2.0 * DEPTH) ** 0.25
DEEPNORM_BETA = (8.0 * DEPTH) ** -0.25
IN_SPLITS = (FOX_WIDTH, FOX_WIDTH, FOX_WIDTH, FOX_HEADS, GLA_KEY_WIDTH, GLA_KEY_WIDTH, GLA_WIDTH, GLA_GATE_RANK, GLA_WIDTH)
IN_SCALES = (1.0, 1.0, DEEPNORM_BETA, 1.0, 1.0, 1.0, DEEPNORM_BETA, 1.0, 1.0)
IN_WIDTH = sum(IN_SPLITS)

kernel_name = 'hybrid_fox_gla_moe_deepnorm_layer'


def layer_norm(x, g, b):
    xf = x.astype(jnp.float32)
    mu = jnp.mean(xf, axis=-1, keepdims=True)
    xc = xf - mu
    var = jnp.mean(xc * xc, axis=-1, keepdims=True)
    return (xc * lax.rsqrt(var + LN_EPS) * g + b).astype(x.dtype)


def rms_norm(x, g):
    xf = x.astype(jnp.float32)
    return xf * lax.rsqrt(jnp.mean(xf * xf, axis=-1, keepdims=True) + LN_EPS) * g


def forgetting_attention(q, k, v, log_f):
    B, S, H, Dh = q.shape
    scale = Dh ** -0.5
    c = jnp.cumsum(log_f, axis=1).transpose(0, 2, 1)
    outs = []
    for i in range(S // Q_BLOCK):
        q0, q1 = i * Q_BLOCK, (i + 1) * Q_BLOCK
        logits = jnp.einsum('bqhd,bkhd->bhqk', q[:, q0:q1], k[:, :q1]).astype(jnp.float32) * scale
        logits = logits + (c[:, :, q0:q1, None] - c[:, :, None, :q1])
        causal = jnp.arange(q0, q1)[:, None] >= jnp.arange(q1)[None, :]
        p = jax.nn.softmax(jnp.where(causal, logits, -jnp.inf), axis=-1)
        outs.append(jnp.einsum('bhqk,bkhd->bqhd', p.astype(v.dtype), v[:, :q1]))
    return jnp.concatenate(outs, axis=1)


def gla_chunked(q, k, v, log_a):
    B, S, H, Dk = q.shape
    Dv = v.shape[-1]
    N = S // CHUNK

    def to_chunks(t):
        return t.astype(jnp.float32).reshape(B, N, CHUNK, H, -1).transpose(1, 0, 3, 2, 4)

    qc = to_chunks(q) * (Dk ** -0.5)
    kc = to_chunks(k)
    vc = to_chunks(v)
    lac = to_chunks(log_a)
    causal = jnp.tril(jnp.ones((CHUNK, CHUNK), dtype=bool))

    def step(state, inp):
        qi, ki, vi, lai = inp
        b = jnp.cumsum(lai, axis=2)
        o_inter = jnp.einsum('bhtd,bhde->bhte', qi * jnp.exp(b), state)
        diff = b[:, :, :, None, :] - b[:, :, None, :, :]
        decay = jnp.exp(jnp.where(causal[:, :, None], diff, -jnp.inf))
        scores = jnp.einsum('bhtd,bhsd,bhtsd->bhts', qi, ki, decay)
        o_intra = jnp.einsum('bhts,bhse->bhte', scores, vi)
        b_last = b[:, :, -1:, :]
        new_state = (jnp.exp(b_last[:, :, 0, :, None]) * state
                     + jnp.einsum('bhsd,bhse->bhde', ki * jnp.exp(b_last - b), vi))
        return new_state, o_inter + o_intra

    state0 = jnp.zeros((B, H, Dk, Dv), jnp.float32)
    _, o = lax.scan(step, state0, (qc, kc, vc, lac))
    return o.transpose(1, 0, 3, 2, 4).reshape(B, S, H, Dv)


def hybrid_mixer(h, w_in, b_fgate, w_gate_up, b_gate, g_gla_norm, w_out):
    B, S, _ = h.shape
    proj = jnp.einsum('bsd,de->bse', h, w_in)
    fq, fk, fv, ff, gq, gk, gv, ga, gr = jnp.split(proj, np.cumsum(IN_SPLITS)[:-1].tolist(), axis=-1)
    log_f = jax.nn.log_sigmoid((ff + b_fgate).astype(jnp.float32))
    y_fox = forgetting_attention(fq.reshape(B, S, FOX_HEADS, FOX_HEAD_DIM),
                                 fk.reshape(B, S, FOX_HEADS, FOX_HEAD_DIM),
                                 fv.reshape(B, S, FOX_HEADS, FOX_HEAD_DIM), log_f)
    y_fox = y_fox.reshape(B, S, FOX_WIDTH).astype(h.dtype)
    gate_logits = jnp.einsum('bsr,rk->bsk', ga, w_gate_up) + b_gate
    log_a = jax.nn.log_sigmoid(gate_logits.astype(jnp.float32)) / GLA_GATE_TEMP
    o = gla_chunked(gq.reshape(B, S, GLA_HEADS, GLA_KEY_DIM),
                    gk.reshape(B, S, GLA_HEADS, GLA_KEY_DIM),
                    gv.reshape(B, S, GLA_HEADS, GLA_VALUE_DIM),
                    log_a.reshape(B, S, GLA_HEADS, GLA_KEY_DIM))
    o = rms_norm(o, g_gla_norm) * jax.nn.silu(gr.astype(jnp.float32)).reshape(B, S, GLA_HEADS, GLA_VALUE_DIM)
    y_gla = o.reshape(B, S, GLA_WIDTH).astype(h.dtype)
    y = jnp.concatenate([y_fox, y_gla], axis=-1)
    return jnp.einsum('bse,ed->bsd', y, w_out)


def swiglu(x, wg, wu, wd):
    return (jax.nn.silu(x @ wg) * (x @ wu)) @ wd


def moe_ffn(h, w_router, router_bias, w_exp_gate, w_exp_up, w_exp_down, w_sh_gate, w_sh_up, w_sh_down):
    B, S, D = h.shape
    xf = h.reshape(-1, D)
    T = B * S
    scores = jax.nn.sigmoid(jnp.einsum('td,de->te', xf.astype(jnp.float32), w_router.astype(jnp.float32)))
    biased = scores + router_bias.astype(jnp.float32)
    grp_score = lax.top_k(biased.reshape(T, N_GROUPS, N_EXPERTS // N_GROUPS), 2)[0].sum(-1)
    _, grp_idx = lax.top_k(grp_score, TOPK_GROUPS)
    grp_mask = jnp.any(grp_idx[:, :, None] == jnp.arange(N_GROUPS)[None, None, :], axis=1)
    expert_mask = jnp.repeat(grp_mask, N_EXPERTS // N_GROUPS, axis=1)
    _, top_idx = lax.top_k(jnp.where(expert_mask, biased, -jnp.inf), TOP_K)
    top_w = jnp.take_along_axis(scores, top_idx, axis=1)
    top_w = top_w / jnp.sum(top_w, axis=-1, keepdims=True) * ROUTED_SCALE
    A = T * TOP_K
    eid = top_idx.reshape(-1).astype(jnp.int32)
    tok = jnp.repeat(jnp.arange(T, dtype=jnp.int32), TOP_K)
    gate = top_w.reshape(-1)
    order = jnp.argsort(eid)
    eid_s, tok_s, gate_s = eid[order], tok[order], gate[order]
    counts = jnp.bincount(eid, length=N_EXPERTS).astype(jnp.int32)
    padded = (counts + EXPERT_ROWS - 1) // EXPERT_ROWS * EXPERT_ROWS
    start = jnp.cumsum(counts) - counts
    pend = jnp.cumsum(padded)
    pstart = pend - padded
    dest = pstart[eid_s] + jnp.arange(A, dtype=jnp.int32) - start[eid_s]
    P = -(-A // EXPERT_ROWS) * EXPERT_ROWS + N_EXPERTS * EXPERT_ROWS
    nb = P // EXPERT_ROWS
    row_tok = jnp.zeros((P,), jnp.int32).at[dest].set(tok_s)
    row_gate = jnp.zeros((P,), jnp.float32).at[dest].set(gate_s)
    blk_exp = jnp.minimum(jnp.searchsorted(pend, jnp.arange(nb, dtype=jnp.int32) * EXPERT_ROWS, side='right'),
                          N_EXPERTS - 1)

    def body(acc, inp):
        rt, rg, e = inp
        y = swiglu(xf[rt], w_exp_gate[e], w_exp_up[e], w_exp_down[e]).astype(jnp.float32) * rg[:, None]
        return acc.at[rt].add(y), None

    routed, _ = lax.scan(body, jnp.zeros((T, D), jnp.float32),
                         (row_tok.reshape(nb, EXPERT_ROWS), row_gate.reshape(nb, EXPERT_ROWS), blk_exp))
    shared = swiglu(xf, w_sh_gate, w_sh_up, w_sh_down).astype(jnp.float32)
    return (routed + shared).astype(h.dtype).reshape(B, S, D)


def setup_inputs(seed: int = 0) -> dict:
    key = jax.random.key(seed)
    ks = jax.random.split(key, 24)
    L, D = DEPTH, D_MODEL

    def nrm(k, shape, scale):
        return jax.random.normal(k, shape, jnp.float32) * scale

    col_scale = jnp.asarray(np.concatenate([np.full((n,), s, np.float32) for n, s in zip(IN_SPLITS, IN_SCALES)]))
    return {
        'x': jax.random.normal(ks[0], (BATCH, SEQ, D), jnp.float32),
        'ln_in_g': 1.0 + nrm(ks[1], (D,), 0.02),
        'ln_in_b': nrm(ks[2], (D,), 0.02),
        'w_in': nrm(ks[3], (L, D, IN_WIDTH), D ** -0.5) * col_scale,
        'b_fgate': jax.random.uniform(ks[4], (L, FOX_HEADS), jnp.float32, 1.0, 4.0),
        'w_gate_up': nrm(ks[5], (L, GLA_GATE_RANK, GLA_KEY_WIDTH), GLA_GATE_RANK ** -0.5),
        'b_gate': nrm(ks[6], (L, GLA_KEY_WIDTH), 0.1),
        'g_gla_norm': 1.0 + nrm(ks[7], (L, GLA_VALUE_DIM), 0.02),
        'w_out': nrm(ks[8], (L, MIX_WIDTH, D), MIX_WIDTH ** -0.5 * DEEPNORM_BETA),
        'ln1_g': 1.0 + nrm(ks[9], (L, D), 0.02),
        'ln1_b': nrm(ks[10], (L, D), 0.02),
        'w_router': nrm(ks[11], (L, D, N_EXPERTS), D ** -0.5),
        'router_bias': nrm(ks[12], (L, N_EXPERTS), 0.01),
        'w_exp_gate': nrm(ks[13], (L, N_EXPERTS, D, EXPERT_DIM), D ** -0.5),
        'w_exp_up': nrm(ks[14], (L, N_EXPERTS, D, EXPERT_DIM), D ** -0.5 * DEEPNORM_BETA),
        'w_exp_down': nrm(ks[15], (L, N_EXPERTS, EXPERT_DIM, D), EXPERT_DIM ** -0.5 * DEEPNORM_BETA),
        'w_sh_gate': nrm(ks[16], (L, D, SHARED_DIM), D ** -0.5),
        'w_sh_up': nrm(ks[17], (L, D, SHARED_DIM), D ** -0.5 * DEEPNORM_BETA),
        'w_sh_down': nrm(ks[18], (L, SHARED_DIM, D), SHARED_DIM ** -0.5 * DEEPNORM_BETA),
        'ln2_g': 1.0 + nrm(ks[19], (L, D), 0.02),
        'ln2_b': nrm(ks[20], (L, D), 0.02),
    }


def reference(x, ln_in_g, ln_in_b, w_in, b_fgate, w_gate_up, b_gate, g_gla_norm, w_out, ln1_g, ln1_b,
              w_router, router_bias, w_exp_gate, w_exp_up, w_exp_down, w_sh_gate, w_sh_up, w_sh_down,
              ln2_g, ln2_b):
    h = layer_norm(x, ln_in_g, ln_in_b)
    for l in range(DEPTH):
        mix = hybrid_mixer(h, w_in[l], b_fgate[l], w_gate_up[l], b_gate[l], g_gla_norm[l], w_out[l])
        h = layer_norm(DEEPNORM_ALPHA * h + mix, ln1_g[l], ln1_b[l])
        ffn = moe_ffn(h, w_router[l], router_bias[l], w_exp_gate[l], w_exp_up[l], w_exp_down[l],
                      w_sh_gate[l], w_sh_up[l], w_sh_down[l])
        h = layer_norm(DEEPNORM_ALPHA * h + ffn, ln2_g[l], ln2_b[l])
    return h
```

```python
import contextlib
import os
import numpy as np
import concourse.bass as bass
import concourse.mybir as mybir
from concourse.bass_utils import run_bass_kernel_spmd

F32 = mybir.dt.float32
BF16 = mybir.dt.bfloat16
U32 = mybir.dt.uint32
U8 = mybir.dt.uint8
AF = mybir.ActivationFunctionType
ALU = mybir.AluOpType
AX = mybir.AxisListType

SEM_WRAP = 30000
NDMASEM = 16
S = 4096
D = 1024
NT = 32
NE = 256
CAP = 256
EPS = 1e-5
ALPHA = 2.0 ** 0.25
ARENA = 184 * 1024


class Buf:
    __slots__ = ("name", "w", "r")

    def __init__(self, name=""):
        self.name = name
        self.w = None
        self.r = []


class Op:
    __slots__ = ("eng", "fn", "dma", "deps", "sig", "seq", "dsem", "dval", "flow", "cidx")

    def __init__(self, eng, fn, dma):
        self.eng = eng
        self.fn = fn
        self.dma = dma
        self.deps = []
        self.sig = False
        self.seq = None
        self.dsem = None
        self.dval = None
        self.flow = None


class Prog:
    ENGS = ("pe", "act", "dve", "pool", "sp")

    def __init__(self, nc):
        self.nc = nc
        self.ops = []
        self.stack = contextlib.ExitStack()
        self.barrier_op = None
        self.last = {}
        self.dmas_since = []

    def buf(self):
        return Buf()

    def bufs(self, n):
        return [Buf() for _ in range(n)]

    stage = 0
    cut = int(os.environ.get("KC_CUT", "99"))

    capture = None
    DIST = 1 << 30

    def cap_begin(self):
        self.capture = []

    def cap_end(self):
        c, self.capture = self.capture, None
        return c

    def play(self, lst, k=None):
        n = len(lst) if k is None else min(k, len(lst))
        for _ in range(n):
            a = lst.pop(0)
            self.add(*a)

    def interleave(self, *lists):
        lists = [l for l in lists if l]
        idx = [0] * len(lists)
        while True:
            best, bk = None, -1
            for k, l in enumerate(lists):
                if idx[k] < len(l):
                    frac = idx[k] / len(l)
                    if best is None or frac < best:
                        best, bk = frac, k
            if bk < 0:
                break
            self.add(*lists[bk][idx[bk]])
            idx[bk] += 1

    def add(self, eng, fn, reads=(), writes=(), dma=False):
        if self.capture is not None:
            self.capture.append((eng, fn, tuple(reads), tuple(writes), dma))
            return None
        op = Op(eng, fn, dma)
        if self.stage > self.cut:
            return op
        seen = {}
        for b in reads:
            if b.w is not None:
                seen[id(b.w)] = (b.w, True)
        for b in writes:
            if b.w is not None and id(b.w) not in seen:
                seen[id(b.w)] = (b.w, False)
            for r in b.r:
                if id(r) not in seen:
                    seen[id(r)] = (r, False)
        seen.pop(id(op), None)
        op.deps = list(seen.values())
        if self.barrier_op is not None:
            op.deps.append((self.barrier_op, True))
        for b in writes:
            b.w = op
            b.r = []
        for b in reads:
            b.r.append(op)
        self.ops.append(op)
        if dma:
            self.dmas_since.append(op)
        else:
            self.last[eng] = op
        return op

    def pe(self, fn, reads=(), writes=()):
        return self.add("pe", fn, reads, writes)

    def act(self, fn, reads=(), writes=()):
        return self.add("act", fn, reads, writes)

    def dve(self, fn, reads=(), writes=()):
        return self.add("dve", fn, reads, writes)

    def pool(self, fn, reads=(), writes=()):
        return self.add("pool", fn, reads, writes)

    def dma(self, eng, fn, reads=(), writes=()):
        return self.add(eng, fn, reads, writes, dma=True)

    def barrier(self, scratch):
        deps = [(o, True) for o in self.last.values()] + [(o, True) for o in self.dmas_since]
        op = Op("dve", lambda e: e.memset(scratch, 0.0), False)
        op.deps = deps
        if self.barrier_op is not None:
            op.deps.append((self.barrier_op, True))
        self.ops.append(op)
        self.barrier_op = op
        self.last = {"dve": op}
        self.dmas_since = []
        return op

    def _skip(self, d, op, raw):
        if d.dma or op.dma:
            return False
        if d.eng != op.eng:
            return False
        return d.eng == "pe"

    def emit(self, final_wait_ops=()):
        nc = self.nc
        st = self.stack
        cc = {e: 0 for e in self.ENGS}
        for op in self.ops:
            if not op.dma:
                cc[op.eng] += 1
            op.cidx = cc[op.eng]
        for op in self.ops:
            for d, raw in op.deps:
                if d.dma or self._skip(d, op, raw):
                    continue
                d.sig = True
        cnt = {e: 0 for e in self.ENGS}
        for op in self.ops:
            if not op.dma and op.sig:
                cnt[op.eng] += 1
                op.seq = cnt[op.eng]
        esems = {}
        for e in self.ENGS:
            n = max(1, (cnt[e] + SEM_WRAP - 1) // SEM_WRAP)
            esems[e] = [st.enter_context(nc.semaphore(f"s_{e}{i}")) for i in range(n)]
        dcnt = {}
        dsems = {}
        dlast = {}
        for op in self.ops:
            if op.dma:
                e = op.eng
                if e not in dsems:
                    dsems[e] = [st.enter_context(nc.semaphore(f"d_{e}{i}")) for i in range(NDMASEM)]
                    dlast[e] = [None] * NDMASEM
                    dcnt[e] = 0
                n = dcnt[e]
                dcnt[e] += 1
                k = n % NDMASEM
                op.dsem = dsems[e][k]
                op.dval = 16 * (n // NDMASEM + 1)
                op.flow = dlast[e][k]
                dlast[e][k] = op

        def target(d):
            if d.dma:
                return d.dsem, d.dval
            s = (d.seq - 1) // SEM_WRAP
            return esems[d.eng][s], (d.seq - 1) % SEM_WRAP + 1

        per = {e: [] for e in self.ENGS}
        for op in self.ops:
            per[op.eng].append(op)
        self.stats = {e: len(per[e]) for e in self.ENGS}
        nw = [0]

        def run_engine(ename, eng):
            waited = {}
            for op in per[ename]:
                need = {}
                deps = list(op.deps)
                if op.flow is not None:
                    deps.append((op.flow, True))
                for d, raw in deps:
                    if self._skip(d, op, raw):
                        continue
                    sem, val = target(d)
                    key = sem.num
                    if waited.get(key, 0) >= val:
                        continue
                    if key not in need or need[key][1] < val:
                        need[key] = (sem, val)
                for key, (sem, val) in need.items():
                    waited[key] = val
                    eng.wait_ge(sem, val)
                    nw[0] += 1
                ins = op.fn(eng)
                if op.dma:
                    ins.then_inc(op.dsem, 16)
                elif op.sig:
                    ins.then_inc(target(op)[0], 1)
            if ename == "sp":
                for op in final_wait_ops:
                    sem, val = target(op)
                    eng.wait_ge(sem, val)

        with nc.Block() as block:
            @block.tensor
            def _(e):
                run_engine("pe", e)

            @block.scalar
            def _(e):
                run_engine("act", e)

            @block.vector
            def _(e):
                run_engine("dve", e)

            @block.gpsimd
            def _(e):
                run_engine("pool", e)

            @block.sync
            def _(e):
                run_engine("sp", e)
        self.stats["waits"] = nw[0]
        self.stack.close()


_DS = {F32: 4, BF16: 2, U32: 4, U8: 1}

_BREG = {}


def _breg(e, val):
    k = (id(e), val)
    if k not in _BREG:
        _BREG[k] = e.to_reg(val)
    return _BREG[k]


class Arena:
    def __init__(self, t, size):
        self.t = t
        self.size = size
        self.off = 0
        self.peak = 0

    def alloc(self, shape, dtype):
        n = 1
        for s in shape[1:]:
            n *= s
        nb = n * _DS[dtype]
        off = self.off
        self.off += (nb + 63) // 64 * 64
        self.peak = max(self.peak, self.off)
        assert self.off <= self.size, f"arena overflow {self.off}"
        v = self.t[:, off:off + nb].bitcast(dtype)
        if shape[0] < 128:
            v = v[0:shape[0], :]
        if len(shape) == 3:
            v = v.rearrange("p (a b) -> p a b", a=shape[1])
        elif len(shape) == 4:
            v = v.rearrange("p (a b c) -> p a b c", a=shape[1], b=shape[2])
        return v

    def mark(self):
        return self.off

    def release(self, m):
        self.off = m


C_ID, C_LE, C_LT, C_IOTA, C_TOK, C_END = 0, 128, 256, 384, 640, 672

IN_NAMES = ["x", "cst", "slot_init", "lnfm", "ln_in_g", "ln_in_b", "w_in", "b_fgate", "w_gate_up", "b_gate", "g_gla_norm", "w_out",
            "ln1_g", "ln1_b", "w_router", "router_bias", "w_exp_gate", "w_exp_up", "w_exp_down",
            "w_sh_gate", "w_sh_up", "w_sh_down", "ln2_g", "ln2_b"]


def build(stop_after="F", debug=False):
    nc = bass.Bass("TRN2", target_bir_lowering=False)
    P = Prog(nc)

    def din(name, shape, dt=F32):
        return nc.dram_tensor(name, list(shape), dt, kind="ExternalInput").ap()

    skind = "ExternalOutput" if debug else "Internal"

    def dscr(name, shape, dt):
        return nc.dram_tensor(name, list(shape), dt, kind=skind).ap()

    x = din("x", [S, D])
    cst_d = din("cst", [128, C_END])
    slot_init_d = din("slot_init", [128, 2048], U32)
    lnfm_d = din("lnfm", [128, 16])
    ln_in_g = din("ln_in_g", [D]); ln_in_b = din("ln_in_b", [D])
    w_in = din("w_in", [D, 3096])
    b_fgate = din("b_fgate", [8])
    w_gate_up = din("w_gate_up", [16, 256]); b_gate = din("b_gate", [256])
    g_gla_norm = din("g_gla_norm", [128])
    w_out = din("w_out", [D, D])
    ln1_g = din("ln1_g", [D]); ln1_b = din("ln1_b", [D])
    w_router = din("w_router", [D, 256]); router_bias = din("router_bias", [256])
    w_exp_gate = din("w_exp_gate", [NE, D, 256]); w_exp_up = din("w_exp_up", [NE, D, 256])
    w_exp_down = din("w_exp_down", [NE, 256, D])
    w_sh_gate = din("w_sh_gate", [D, 256]); w_sh_up = din("w_sh_up", [D, 256]); w_sh_down = din("w_sh_down", [256, D])
    ln2_g = din("ln2_g", [D]); ln2_b = din("ln2_b", [D])
    out = nc.dram_tensor("out", [S, D], F32, kind="ExternalOutput").ap()

    qk_s = dscr("qk_s", [8, 2, 70, S], BF16)
    v_s = dscr("v_s", [S, 8, 65], BF16)
    gq_s = dscr("gq_s", [256, S], F32)
    gk_s = dscr("gk_s", [256, S], F32)
    la_s = dscr("la_s", [256, S], F32)
    gv_s = dscr("gv_s", [S, 512], BF16)
    gr_s = dscr("gr_s", [S, 512], BF16)
    y_s = dscr("y_s", [S, D], BF16)
    h2_s = dscr("h2_s", [S, D], F32)
    h2b_s = dscr("h2b_s", [S, D], BF16)
    ysh_s = dscr("ysh_s", [S, D], F32)
    slot_s = dscr("slot_s", [NE * CAP, 4], U32)
    Y_s = dscr("Y_s", [NE * CAP, D], BF16)
    dbg_dest = dscr("dbg_dest", [128, NT * 8], U32) if debug else None
    NOSCAT = bool(os.environ.get("KC_NOSCAT"))

    arena_t = P.stack.enter_context(nc.sbuf_tensor("arena", [128, ARENA], U8))
    ar = Arena(arena_t, ARENA)
    ps = [P.stack.enter_context(nc.psum_tensor(f"ps{i}", [128, 512], F32)) for i in range(8)]
    Bps = P.bufs(8)

    def psb(i):
        return ps[i][:, :].bitcast(BF16)

    cst = ar.alloc([128, C_END], F32)
    ident_bf = ar.alloc([128, 128], BF16)
    le_bf = ar.alloc([128, 128], BF16)
    lt_bf = ar.alloc([128, 128], BF16)
    ones_bf = ar.alloc([128, 128], BF16)
    destall = ar.alloc([128, NT, 8], U32)
    bscr = ar.alloc([128, 16], F32)
    Bcst = P.buf(); Bdest = P.bufs(NT)
    P.dma("sp", lambda e: e.dma_start(out=cst, in_=cst_d), writes=[Bcst])
    P.dve(lambda e: e.tensor_copy(out=ident_bf, in_=cst[:, C_ID:C_ID + 128]), reads=[Bcst], writes=[Bcst])
    P.dve(lambda e: e.tensor_copy(out=le_bf, in_=cst[:, C_LE:C_LE + 128]), reads=[Bcst], writes=[Bcst])
    P.dve(lambda e: e.tensor_copy(out=lt_bf, in_=cst[:, C_LT:C_LT + 128]), reads=[Bcst], writes=[Bcst])
    P.dve(lambda e: e.memset(ones_bf, 1.0), writes=[Bcst])
    ident_f = cst[:, C_ID:C_ID + 128]
    iota_f = cst[:, C_IOTA:C_IOTA + 256]
    tok_f = cst[:, C_TOK:C_TOK + 32]
    gmark = ar.mark()
    outs = []

    def ln_stats(xt, mv, stt, rstd, Bx, Bmv):
        for hf in range(2):
            P.dve(lambda e, hf=hf: e.bn_stats(out=stt[:, hf, :], in_=xt[:, hf * 512:(hf + 1) * 512]), reads=[Bx], writes=[Bmv])
        P.dve(lambda e: e.bn_aggr(out=mv[:, 0:2], in_=stt.rearrange("p a b -> p (a b)")), reads=[Bmv], writes=[Bmv])
        P.act(lambda e: e.activation(out=rstd, in_=mv[:, 1:2], func=AF.Ln, bias=epsb[:, 0:1], scale=1.0), reads=[Bmv], writes=[Bmv])
        P.act(lambda e: e.activation(out=rstd, in_=rstd, func=AF.Exp, scale=-0.5), reads=[Bmv], writes=[Bmv])

    epsb = ar.alloc([128, 1], F32)
    oneb = ar.alloc([128, 1], F32)
    P.dve(lambda e: e.memset(epsb, EPS), writes=[Bcst])
    P.dve(lambda e: e.memset(oneb, 1.0), writes=[Bcst])
    gmark = ar.mark()

    def phase_A():
        logf = ar.alloc([8, S], F32)
        m2 = ar.mark()
        win = ar.alloc([128, 8, 3096], BF16)
        lnfm = ar.alloc([128, 16], F32)
        hT = [ar.alloc([128, 8, 512], BF16) for _ in range(2)]
        xts = [ar.alloc([128, 1024], F32) for _ in range(3)]
        xhs = [ar.alloc([128, 1024], BF16) for _ in range(2)]
        stts = [ar.alloc([128, 2, 6], F32) for _ in range(3)]
        mvs = [ar.alloc([128, 4], F32) for _ in range(3)]
        qst = [ar.alloc([64, 8, 512], BF16) for _ in range(2)]
        kst = [ar.alloc([64, 8, 512], BF16) for _ in range(2)]
        gst = [ar.alloc([128, 512], F32) for _ in range(4)]
        vst = [ar.alloc([128, 8, 65], BF16) for _ in range(2)]
        gvst = [ar.alloc([128, 512], BF16) for _ in range(2)]
        grst = [ar.alloc([128, 512], BF16) for _ in range(2)]
        gaT = [ar.alloc([17, 512], F32) for _ in range(2)]
        wgu = ar.alloc([17, 256], F32)
        nbf = ar.alloc([8, 1], F32)
        t8 = [ar.alloc([8, 512], F32) for _ in range(2)]
        Bwin = P.buf(); Bln = P.buf(); BhT = [P.bufs(4) for _ in range(2)]
        Bxt = P.bufs(3); Bxh = P.bufs(2); Bmv = P.bufs(3)
        Bq = P.bufs(2); Bk = P.bufs(2); Bg = P.bufs(4); Bv = P.bufs(2); Bgv = P.bufs(2); Bgr = P.bufs(2)
        Blogf = P.buf(); Bga = P.bufs(2); Bwgu = P.buf(); Bt8 = P.bufs(2)

        for c in range(8):
            for j in range(3):
                P.dma("pool", lambda e, c=c, j=j: e.dma_start(out=win[:, c, j * 1032:(j + 1) * 1032],
                                                             in_=w_in[c * 128:(c + 1) * 128, j * 1032:(j + 1) * 1032]), writes=[Bwin])
        P.dma("sp", lambda e: e.dma_start(out=lnfm, in_=lnfm_d), writes=[Bln])
        P.dma("sp", lambda e: e.dma_start(out=wgu[0:16, :], in_=w_gate_up), writes=[Bwgu])
        P.dma("sp", lambda e: e.dma_start(out=wgu[16:17, :], in_=b_gate.rearrange("(o n) -> o n", o=1)), writes=[Bwgu])
        P.dma("sp", lambda e: e.dma_start(out=nbf, in_=b_fgate.rearrange("(p o) -> p o", o=1)), writes=[Bln])
        P.dve(lambda e: e.tensor_scalar(out=nbf, in0=nbf, scalar1=-1.0, scalar2=None, op0=ALU.mult), reads=[Bln], writes=[Bln])
        for i in range(2):
            P.dve(lambda e, i=i: e.memset(gaT[i], 1.0), writes=[Bga[i]])
            P.dve(lambda e, i=i: e.memset(vst[i], 1.0), writes=[Bv[i]])

        bank = [2]

        def nextbank():
            b = bank[0]
            bank[0] = 2 + (bank[0] - 2 + 1) % 6
            return b

        gi = [0]
        def ln_chunk(ch):
            hb = ch % 2
            for t in range(4):
                g = ch * 4 + t
                xt = xts[g % 3]; xh = xhs[g % 2]; mv = mvs[g % 3]; stt = stts[g % 3]
                P.dma("sp" if g < 4 else "pool", lambda e, g=g, xt=xt: e.dma_start(out=xt, in_=x[g * 128:(g + 1) * 128, :]), writes=[Bxt[g % 3]])
                ln_stats(xt, mv, stt, mv[:, 2:3], Bxt[g % 3], Bmv[g % 3])
                P.dve(lambda e, xt=xt, xh=xh, mv=mv: e.tensor_scalar(out=xh, in0=xt, scalar1=mv[:, 0:1], scalar2=mv[:, 2:3],
                                                                     op0=ALU.subtract, op1=ALU.mult),
                      reads=[Bxt[g % 3], Bmv[g % 3]], writes=[Bxh[g % 2]])
                pb = g % 2
                for c in range(8):
                    P.pe(lambda e, c=c, xh=xh, pb=pb: e.transpose(out=psb(pb)[:, c * 128:(c + 1) * 128], in_=xh[:, c * 128:(c + 1) * 128],
                                                                    identity=ident_bf), reads=[Bxh[g % 2], Bcst], writes=[Bps[pb]])
                for c in range(8):
                    P.act(lambda e, c=c, pb=pb, hb=hb, t=t: e.activation(out=hT[hb][:, c, t * 128:(t + 1) * 128],
                                                                          in_=psb(pb)[:, c * 128:(c + 1) * 128], func=AF.Identity,
                                                                          scale=lnfm[:, c:c + 1], bias=lnfm[:, 8 + c:9 + c]),
                          reads=[Bps[pb], Bln], writes=[BhT[hb][t]])

        def proj_chunk(ch):
            hb = ch % 2
            cols = slice(ch * 512, (ch + 1) * 512)

            def fm_group(col0, ncols):
                b = nextbank()
                for c in range(8):
                    P.pe(lambda e, c=c, b=b, hb=hb, col0=col0, ncols=ncols: e.matmul(ps[b][0:ncols, :], lhsT=win[:, c, col0:col0 + ncols], rhs=hT[hb][:, c, :],
                                                      start=(c == 0), stop=(c == 7)),
                         reads=[Bwin] + BhT[hb], writes=[Bps[b]])
                return b

            for which, base, stg, Bst_, sc in (("q", 0, qst, Bq, 0.125), ("k", 512, kst, Bk, 1.0)):
                for i in range(4):
                    b = fm_group(base + i * 128, 128)
                    P.dve(lambda e, b=b, i=i, stg=stg, sc=sc, hb=hb: e.tensor_scalar(out=stg[hb][:, 2 * i, :], in0=ps[b][0:64, :], scalar1=sc,
                                                                               scalar2=None, op0=ALU.mult),
                          reads=[Bps[b]], writes=[Bst_[hb]])
                    P.act(lambda e, b=b, i=i, stg=stg, sc=sc, hb=hb: e.activation(out=stg[hb][:, 2 * i + 1, :], in_=ps[b][64:128, :], func=AF.Copy,
                                                                            scale=sc),
                          reads=[Bps[b]], writes=[Bst_[hb]])
                side = 0 if which == "q" else 1
                P.dma("sp", lambda e, stg=stg, side=side, hb=hb, cols=cols: e.dma_start(out=qk_s[:, side, 0:64, cols].rearrange("h r t -> r h t"), in_=stg[hb]),
                      reads=[Bst_[hb]])
            for base, dst in ((1544, gq_s), (1800, gk_s)):
                for j in range(2):
                    b = fm_group(base + j * 128, 128)
                    k = gi[0] % 4
                    gi[0] += 1
                    P.dve(lambda e, b=b, k=k: e.tensor_copy(out=gst[k], in_=ps[b][:, :]), reads=[Bps[b]], writes=[Bg[k]])
                    P.dma("sp", lambda e, k=k, j=j, dst=dst, cols=cols: e.dma_start(out=dst[j * 128:(j + 1) * 128, cols], in_=gst[k]), reads=[Bg[k]])
            b = fm_group(1536, 8)
            tb = ch % 2
            P.act(lambda e, b=b, tb=tb: e.activation(out=t8[tb], in_=ps[b][0:8, :], func=AF.Exp, scale=-1.0, bias=nbf[:, 0:1]),
                  reads=[Bps[b], Bln], writes=[Bt8[tb]])
            P.act(lambda e, tb=tb: e.activation(out=t8[tb], in_=t8[tb], func=AF.Ln, bias=oneb[0:8, 0:1], scale=1.0), reads=[Bt8[tb]], writes=[Bt8[tb]])
            P.dve(lambda e, tb=tb, cols=cols: e.tensor_scalar(out=logf[:, cols], in0=t8[tb], scalar1=-1.0, scalar2=None, op0=ALU.mult),
                  reads=[Bt8[tb]], writes=[Blogf])
            b = fm_group(2568, 16)
            P.dve(lambda e, b=b, hb=hb: e.tensor_copy(out=gaT[hb][0:16, :], in_=ps[b][0:16, :]), reads=[Bps[b]], writes=[Bga[hb]])
            for j in range(2):
                b = nextbank()
                P.pe(lambda e, b=b, j=j, hb=hb: e.matmul(ps[b][:, :], lhsT=wgu[:, j * 128:(j + 1) * 128], rhs=gaT[hb][:, :], start=True, stop=True),
                     reads=[Bwgu, Bga[hb]], writes=[Bps[b]])
                k = gi[0] % 4
                gi[0] += 1
                P.act(lambda e, b=b, k=k: e.activation(out=gst[k], in_=ps[b][:, :], func=AF.Exp, scale=-1.0), reads=[Bps[b]], writes=[Bg[k]])
                P.act(lambda e, k=k: e.activation(out=gst[k], in_=gst[k], func=AF.Ln, bias=oneb[:, 0:1], scale=1.0), reads=[Bg[k]], writes=[Bg[k]])
                P.dve(lambda e, k=k: e.tensor_scalar(out=gst[k], in0=gst[k], scalar1=-1.0 / 16.0, scalar2=None, op0=ALU.mult),
                      reads=[Bg[k]], writes=[Bg[k]])
                P.dma("sp", lambda e, k=k, j=j, cols=cols: e.dma_start(out=la_s[j * 128:(j + 1) * 128, cols], in_=gst[k]), reads=[Bg[k]])
            for t in range(4):
                g = ch * 4 + t
                rows = slice(g * 128, (g + 1) * 128)
                for col0, kind in ((1024, "fv"), (2056, "gv"), (2584, "gr")):
                    b = nextbank()
                    for c in range(8):
                        P.pe(lambda e, c=c, b=b, t=t, col0=col0, hb=hb: e.matmul(ps[b][:, :], lhsT=hT[hb][:, c, t * 128:(t + 1) * 128],
                                                                           rhs=win[:, c, col0:col0 + 512], start=(c == 0), stop=(c == 7)),
                             reads=[Bwin, BhT[hb][t]], writes=[Bps[b]])
                    k = g % 2
                    if kind == "fv":
                        P.act(lambda e, b=b, k=k: e.activation(out=vst[k][:, :, 0:64], in_=ps[b][:, :].rearrange("p (h d) -> p h d", h=8),
                                                               func=AF.Copy), reads=[Bps[b]], writes=[Bv[k]])
                        P.dma("sp", lambda e, k=k, rows=rows: e.dma_start(out=v_s[rows, :, :], in_=vst[k]), reads=[Bv[k]])
                    elif kind == "gv":
                        P.dve(lambda e, b=b, k=k: e.tensor_copy(out=gvst[k], in_=ps[b][:, :]), reads=[Bps[b]], writes=[Bgv[k]])
                        P.dma("sp", lambda e, k=k, rows=rows: e.dma_start(out=gv_s[rows, :], in_=gvst[k]), reads=[Bgv[k]])
                    else:
                        P.act(lambda e, b=b, k=k: e.activation(out=grst[k], in_=ps[b][:, :], func=AF.Copy), reads=[Bps[b]], writes=[Bgr[k]])
                        P.dma("sp", lambda e, k=k, rows=rows: e.dma_start(out=gr_s[rows, :], in_=grst[k]), reads=[Bgr[k]])
        ln_chunk(0)
        for ch in range(8):
            P.cap_begin()
            if ch + 1 < 8:
                ln_chunk(ch + 1)
            la = P.cap_end()
            P.cap_begin()
            proj_chunk(ch)
            lb = P.cap_end()
            P.interleave(la, lb)
        P.barrier(bscr)
        ar.release(m2)
        ones8 = ar.alloc([8, S], BF16)
        cT = ar.alloc([8, S], F32)
        r1 = ar.alloc([8, S], F32)
        parts = [ar.alloc([8, S], BF16) for _ in range(3)]
        negs = [ar.alloc([8, S], BF16) for _ in range(3)]
        Bc = P.buf(); Bpart = P.bufs(3); Bneg = P.bufs(3); Bone = P.buf()
        P.dve(lambda e: e.memset(ones8, 1.0), writes=[Bone])
        P.dve(lambda e: e.tensor_tensor_scan(out=cT, data0=ones8, data1=logf, initial=0.0, op0=ALU.mult, op1=ALU.add),
              reads=[Blogf, Bone], writes=[Bc])
        src = cT
        for i in range(3):
            P.dve(lambda e, i=i, src=src: e.tensor_copy(out=parts[i], in_=src), reads=[Bc], writes=[Bpart[i]])
            if i < 2:
                dst = r1
                P.dve(lambda e, i=i, src=src, dst=dst: e.tensor_tensor(out=dst, in0=src, in1=parts[i], op=ALU.subtract),
                      reads=[Bc, Bpart[i]], writes=[Bc])
                src = r1
            P.dve(lambda e, i=i: e.tensor_scalar(out=negs[i], in0=parts[i], scalar1=-1.0, scalar2=None, op0=ALU.mult),
                  reads=[Bpart[i]], writes=[Bneg[i]])
        for r in range(6):
            qsrc, qb = (parts[r], Bpart[r]) if r < 3 else (ones8, Bone)
            ksrc, kb = (ones8, Bone) if r < 3 else (negs[r - 3], Bneg[r - 3])
            P.dma("sp", lambda e, r=r, qsrc=qsrc: e.dma_start(out=qk_s[:, 0, 64 + r, :], in_=qsrc), reads=[qb])
            P.dma("sp", lambda e, r=r, ksrc=ksrc: e.dma_start(out=qk_s[:, 1, 64 + r, :], in_=ksrc), reads=[kb])
        P.barrier(bscr)
        ar.release(gmark)

    def phase_B():
        qT = [ar.alloc([70, S], BF16) for _ in range(2)]
        kT = [ar.alloc([70, S], BF16) for _ in range(2)]
        vh = [ar.alloc([128, NT, 65], BF16) for _ in range(2)]
        pT = [ar.alloc([128, 512], BF16) for _ in range(3)]
        yh = [ar.alloc([128, NT, 64], BF16) for _ in range(2)]
        rec = [ar.alloc([128, 4], F32) for _ in range(2)]
        Bq = P.bufs(2); Bk = P.bufs(2); Bv = P.bufs(2); BpT = P.bufs(3); Byh = P.bufs(2); Brec = P.bufs(2)
        v_view = v_s.rearrange("(n p) h e -> p n h e", p=128)
        y_view = y_s.rearrange("(n p) c -> p n c", p=128)
        SB = [0, 1, 4]
        units = [(h, c, j) for h in range(8) for c in range(8) for j in range(4 * c + 4)]

        def geom(u):
            h, c, j = u
            i_lo = max(4 * c, j)
            nblk = 4 * c + 4 - i_lo
            return i_lo, nblk, nblk * 128

        def emit_load(h):
            hb = h % 2
            P.dma("sp", lambda e, h=h, hb=hb: e.dma_start(out=qT[hb], in_=qk_s[h, 0, :, :]), writes=[Bq[hb]])
            P.dma("sp", lambda e, h=h, hb=hb: e.dma_start(out=kT[hb], in_=qk_s[h, 1, :, :]), writes=[Bk[hb]])
            for q4 in range(4):
                P.dma("sp", lambda e, h=h, hb=hb, q4=q4: e.dma_start(out=vh[hb][:, q4 * 8:(q4 + 1) * 8, :],
                                                                    in_=v_view[:, q4 * 8:(q4 + 1) * 8, h, :]), writes=[Bv[hb]])

        def emit_mm(u, k):
            h, c, j = u
            hb = h % 2
            i_lo, nblk, ncols = geom(u)
            sb = SB[k % 3]
            P.pe(lambda e, hb=hb, j=j, i_lo=i_lo, ncols=ncols, sb=sb: e.matmul(
                ps[sb][:, 0:ncols], lhsT=kT[hb][:, j * 128:(j + 1) * 128], rhs=qT[hb][:, i_lo * 128:i_lo * 128 + ncols],
                start=True, stop=True), reads=[Bq[hb], Bk[hb]], writes=[Bps[sb]])

        def emit_rest(u, k):
            h, c, j = u
            hb = h % 2
            i_lo, nblk, ncols = geom(u)
            sb = SB[k % 3]
            pb = k % 3
            iu = h * 8 + c
            ab = 2 + iu % 2
            rb = iu % 2
            acc = ps[ab][:, 0:260].rearrange("p (b e) -> p b e", b=4)
            P.act(lambda e, sb=sb, pb=pb, ncols=ncols: e.activation(out=pT[pb][:, 0:ncols], in_=ps[sb][:, 0:ncols], func=AF.Exp),
                  reads=[Bps[sb]], writes=[BpT[pb]])
            if j >= 4 * c:
                P.dve(lambda e, pb=pb: e.tensor_tensor(out=pT[pb][:, 0:128], in0=pT[pb][:, 0:128], in1=le_bf, op=ALU.mult),
                      reads=[BpT[pb], Bcst], writes=[BpT[pb]])
            for bi in range(nblk):
                i = i_lo + bi
                blk = i - 4 * c
                P.pe(lambda e, pb=pb, bi=bi, blk=blk, hb=hb, j=j, acc=acc, first=(j == 0 and blk == 0), last=(j == i): e.matmul(
                    acc[:, blk, :], lhsT=pT[pb][:, bi * 128:(bi + 1) * 128], rhs=vh[hb][:, j, :],
                    start=first, stop=last, skip_group_check=True), reads=[BpT[pb], Bv[hb]], writes=[Bps[ab]])
            if j == 4 * c + 3:
                P.dve(lambda e, acc=acc, rb=rb: e.reciprocal(out=rec[rb], in_=acc[:, :, 64]), reads=[Bps[ab]], writes=[Brec[rb]])
                for blk in range(4):
                    P.dve(lambda e, acc=acc, rb=rb, blk=blk, hb=hb, c=c: e.tensor_scalar(
                        out=yh[hb][:, 4 * c + blk, :], in0=acc[:, blk, 0:64], scalar1=rec[rb][:, blk:blk + 1], scalar2=None, op0=ALU.mult),
                        reads=[Bps[ab], Brec[rb]], writes=[Byh[hb]])
                if c == 7:
                    for q4 in range(4):
                        P.dma("sp", lambda e, h=h, hb=hb, q4=q4: e.dma_start(out=y_view[:, q4 * 8:(q4 + 1) * 8, h * 64:(h + 1) * 64],
                                                                            in_=yh[hb][:, q4 * 8:(q4 + 1) * 8, :]), reads=[Byh[hb]])

        emit_load(0)
        emit_load(1)
        emit_mm(units[0], 0)
        emit_mm(units[1], 1)
        for k, u in enumerate(units):
            if k + 2 < len(units):
                emit_mm(units[k + 2], k + 2)
            emit_rest(u, k)
            if u[1] == 7 and u[2] == 31 and u[0] + 2 < 8:
                emit_load(u[0] + 2)
        P.barrier(bscr)
        ar.release(gmark)

    def phase_C():
        BL = 512
        resetm8 = ar.alloc([64, 8, 64], BF16)
        mask8 = ar.alloc([64, 8, 64], BF16)
        gnb = ar.alloc([64, 128], F32)
        Bm = P.buf()
        P.dve(lambda e: e.memset(resetm8, 1.0), writes=[Bm])
        P.dve(lambda e: e.memset(resetm8[:, :, 0:1], 0.0), writes=[Bm])
        for q in range(8):
            P.dve(lambda e, q=q: e.tensor_copy(out=mask8[:, q, :], in_=le_bf[0:64, 0:64]), reads=[Bcst], writes=[Bm])
        P.dma("sp", lambda e: e.dma_start(out=gnb, in_=g_gla_norm.partition_broadcast(64)), writes=[Bm])
        gv_view = gv_s.rearrange("(n s) e -> s n e", s=64)
        gr_view = gr_s.rearrange("(n s) e -> s n e", s=64)
        y_view = y_s.rearrange("(n s) c -> s n c", s=64)
        H = []
        for hg in range(4):
            d = dict(S1=ar.alloc([64, BL], F32), S2=ar.alloc([64, BL], F32), S3=ar.alloc([64, BL], F32), S4=ar.alloc([64, BL], F32),
                     qt=ar.alloc([64, BL], BF16), kt=ar.alloc([64, BL], BF16), khT=ar.alloc([64, 8, 64], BF16),
                     khat=ar.alloc([64, 8, 64], BF16), gvp=ar.alloc([64, 8, 128], BF16), kvall=ar.alloc([64, 8, 128], F32),
                     ebl=ar.alloc([64, 8], F32), ost=[ar.alloc([64, 8, 128], F32) for _ in range(2)], grp=[ar.alloc([64, 8, 128], BF16) for _ in range(2)],
                     sq=ar.alloc([64, 1024], F32), sg=ar.alloc([64, 1024], F32), ss=ar.alloc([64, 8], F32),
                     yg=ar.alloc([64, 8, 128], BF16), state=ar.alloc([64, 128], F32),
                     sbf=[ar.alloc([64, 128], BF16) for _ in range(2)])
            d["B"] = {k: P.buf() for k in ("S1", "S2", "S3", "S4", "qt", "kt", "khT", "khat", "gvp", "kvall", "ebl", "ost0", "ost1", "grp0", "grp1", "sq", "sg",
                                             "ss", "yg", "state", "sbf0", "sbf1")}
            H.append(d)
            P.dve(lambda e, d=d: e.memset(d["state"], 0.0), writes=[d["B"]["state"]])
            P.dve(lambda e, d=d: e.memset(d["sbf"][0], 0.0), writes=[d["B"]["sbf0"]])

        def head_block(hg, bk, part):
            d = H[hg]; B = d["B"]
            par = bk % 2
            Bost = B["ost%d" % par]; Bgrp = B["grp%d" % par]
            S1, S2, S3, S4, qt, kt, khT, khat, gvp, kvall = (d[k] for k in ("S1", "S2", "S3", "S4", "qt", "kt", "khT", "khat", "gvp", "kvall"))
            ebl, sq, sg, ss, yg, state, sbf = (d[k] for k in ("ebl", "sq", "sg", "ss", "yg", "state", "sbf"))
            ost = d["ost"][par]; grp = d["grp"][par]
            Bsbf = [B["sbf0"], B["sbf1"]]
            rows = slice(hg * 64, (hg + 1) * 64)
            es = slice(hg * 128, (hg + 1) * 128)
            cols = slice(bk * BL, (bk + 1) * BL)
            ns = slice(bk * 8, (bk + 1) * 8)
            pa, pb = 2 * hg, 2 * hg + 1
            def post():
                o = ost.rearrange("p a b -> p (a b)")
                grf = grp.rearrange("p a b -> p (a b)")
                for a8 in range(8):
                    P.act(lambda e, a8=a8: e.activation(out=sq[:, a8 * 128:(a8 + 1) * 128], in_=o[:, a8 * 128:(a8 + 1) * 128], func=AF.Square,
                                                        accum_out=ss[:, a8:a8 + 1]), reads=[Bost], writes=[B["sq"], B["ss"]])
                P.act(lambda e: e.activation(out=ss, in_=ss, func=AF.Ln, scale=1.0 / 128.0, bias=epsb[0:64, 0:1]), reads=[B["ss"]], writes=[B["ss"]])
                P.act(lambda e: e.activation(out=ss, in_=ss, func=AF.Exp, scale=-0.5), reads=[B["ss"]], writes=[B["ss"]])
                P.act(lambda e: e.activation(out=sg, in_=grf, func=AF.Exp, scale=-1.0), reads=[Bgrp], writes=[B["sg"]])
                P.dve(lambda e: e.tensor_scalar(out=sg, in0=sg, scalar1=1.0, scalar2=None, op0=ALU.add), reads=[B["sg"]], writes=[B["sg"]])
                P.dve(lambda e: e.reciprocal(out=sg, in_=sg), reads=[B["sg"]], writes=[B["sg"]])
                P.dve(lambda e: e.tensor_tensor(out=sg, in0=sg, in1=grf, op=ALU.mult), reads=[B["sg"], Bgrp], writes=[B["sg"]])
                P.dve(lambda e: e.tensor_tensor(out=sg.rearrange("p (a b) -> p a b", b=128), in0=sg.rearrange("p (a b) -> p a b", b=128),
                                                in1=gnb.unsqueeze(1).to_broadcast([64, 8, 128]), op=ALU.mult), reads=[B["sg"], Bm], writes=[B["sg"]])
                P.dve(lambda e: e.tensor_tensor(out=sq.rearrange("p (a b) -> p a b", b=128), in0=o.rearrange("p (a b) -> p a b", b=128),
                                                in1=ss.unsqueeze(2).to_broadcast([64, 8, 128]), op=ALU.mult), reads=[Bost, B["ss"]], writes=[B["sq"]])
                P.dve(lambda e: e.tensor_tensor(out=yg.rearrange("p a b -> p (a b)"), in0=sq, in1=sg, op=ALU.mult), reads=[B["sq"], B["sg"]], writes=[B["yg"]])
                P.dma("sp", lambda e: e.dma_start(out=y_view[:, ns, 512 + hg * 128:512 + (hg + 1) * 128], in_=yg), reads=[B["yg"]])

            if part == "post":
                return post()
            P.dma("pool", lambda e: e.dma_start(out=S1, in_=gq_s[rows, cols]), writes=[B["S1"]])
            P.dma("pool", lambda e: e.dma_start(out=S2, in_=gk_s[rows, cols]), writes=[B["S2"]])
            P.dma("pool", lambda e: e.dma_start(out=S3, in_=la_s[rows, cols]), writes=[B["S3"]])
            P.dma("pool", lambda e: e.dma_start(out=gvp, in_=gv_view[:, ns, es]), writes=[B["gvp"]])
            P.dma("pool", lambda e: e.dma_start(out=grp, in_=gr_view[:, ns, es]), writes=[Bgrp])
            S4v = S4.rearrange("p (n t) -> p n t", t=64)
            ktv = kt.rearrange("p (n t) -> p n t", t=64)
            P.dve(lambda e: e.tensor_tensor_scan(out=S4, data0=resetm8.rearrange("p a b -> p (a b)"), data1=S3, initial=0.0,
                                                 op0=ALU.mult, op1=ALU.add), reads=[B["S3"], Bm], writes=[B["S4"]])
            P.act(lambda e: e.activation(out=ebl, in_=S4v[:, :, 63], func=AF.Exp), reads=[B["S4"]], writes=[B["ebl"]])
            P.act(lambda e: e.activation(out=S3, in_=S4, func=AF.Exp), reads=[B["S4"]], writes=[B["S3"]])
            P.dve(lambda e: e.scalar_tensor_tensor(out=qt, in0=S1, scalar=0.125, in1=S3, op0=ALU.mult, op1=ALU.mult),
                  reads=[B["S1"], B["S3"]], writes=[B["qt"]])
            P.act(lambda e: e.activation(out=S3, in_=S4, func=AF.Exp, scale=-1.0), reads=[B["S4"]], writes=[B["S3"]])
            P.dve(lambda e: e.tensor_tensor(out=kt, in0=S2, in1=S3, op=ALU.mult), reads=[B["S2"], B["S3"]], writes=[B["kt"]])
            P.dve(lambda e: e.tensor_tensor(out=khT, in0=ktv, in1=ebl.unsqueeze(2).to_broadcast([64, 8, 64]), op=ALU.mult),
                  reads=[B["kt"], B["ebl"]], writes=[B["khT"]])
            for n in range(8):
                P.pe(lambda e, n=n: e.transpose(out=psb(pa)[0:64, n * 64:(n + 1) * 64], in_=khT[:, n, :], identity=ident_bf[0:64, 0:64]),
                     reads=[B["khT"], Bcst], writes=[Bps[pa]])
            P.act(lambda e: e.activation(out=khat.rearrange("p a b -> p (a b)"), in_=psb(pa)[0:64, 0:512], func=AF.Copy),
                  reads=[Bps[pa]], writes=[B["khat"]])
            for n in range(8):
                cs = slice(n * 64, (n + 1) * 64)
                P.pe(lambda e, n=n, cs=cs: e.matmul(ps[pa][0:64, n * 64:(n + 1) * 64], lhsT=kt[:, cs], rhs=qt[:, cs], start=True, stop=True,
                                                    skip_group_check=True), reads=[B["qt"], B["kt"]], writes=[Bps[pa]])
            P.dve(lambda e: e.tensor_tensor(out=khT, in0=ps[pa][0:64, 0:512].rearrange("p (a b) -> p a b", b=64), in1=mask8, op=ALU.mult),
                  reads=[Bps[pa], Bm], writes=[B["khT"]])
            for i in range(2):
                for q in range(4):
                    n = 4 * i + q
                    P.pe(lambda e, n=n, q=q: e.matmul(ps[pb][0:64, q * 128:(q + 1) * 128], lhsT=khat[:, n, :], rhs=gvp[:, n, :], start=True, stop=True,
                                                      skip_group_check=True), reads=[B["khat"], B["gvp"]], writes=[Bps[pb]])
                P.act(lambda e, i=i: e.activation(out=kvall[:, 4 * i:4 * i + 4, :].rearrange("p a b -> p (a b)"), in_=ps[pb][0:64, :], func=AF.Copy),
                      reads=[Bps[pb]], writes=[B["kvall"]])
            for n in range(8):
                cs = slice(n * 64, (n + 1) * 64)
                ob = pa if n % 2 == 0 else pb
                P.pe(lambda e, n=n, ob=ob: e.matmul(ps[ob][0:64, 0:128], lhsT=khT[:, n, :], rhs=gvp[:, n, :], start=True, stop=False),
                     reads=[B["khT"], B["gvp"]], writes=[Bps[ob]])
                P.pe(lambda e, n=n, cs=cs, ob=ob: e.matmul(ps[ob][0:64, 0:128], lhsT=qt[:, cs], rhs=sbf[n % 2], start=False, stop=True),
                     reads=[B["qt"], Bsbf[n % 2]], writes=[Bps[ob]])
                P.act(lambda e, n=n, ob=ob: e.activation(out=ost[:, n, :], in_=ps[ob][0:64, 0:128], func=AF.Copy), reads=[Bps[ob]], writes=[Bost])
                P.dve(lambda e, n=n: e.scalar_tensor_tensor(out=state, in0=state, scalar=ebl[:, n:n + 1], in1=kvall[:, n, :], op0=ALU.mult, op1=ALU.add),
                      reads=[B["kvall"], B["ebl"], B["state"]], writes=[B["state"]])
                P.act(lambda e, n=n: e.activation(out=sbf[(n + 1) % 2], in_=state, func=AF.Copy), reads=[B["state"]], writes=[Bsbf[(n + 1) % 2]])
        def cap_hb(hg, bk, part):
            P.cap_begin(); head_block(hg, bk, part); return P.cap_end()

        nblk = S // BL
        for bk in range(nblk + 1):
            lists = []
            if bk < nblk:
                lists += [cap_hb(hg, bk, "main") for hg in range(4)]
            if bk > 0:
                lists += [cap_hb(hg, bk - 1, "post") for hg in range(4)]
            P.interleave(*lists)
        P.barrier(bscr)
        ar.release(gmark)

    def bcast_load(dst, vec, B):
        P.dma("sp", lambda e: e.dma_start(out=dst, in_=vec.partition_broadcast(128)), writes=[B])

    def phase_D():
        wout = ar.alloc([128, 8, 1024], BF16)
        g0b = ar.alloc([128, 1024], F32); b0b = ar.alloc([128, 1024], F32)
        g1b = ar.alloc([128, 1024], F32); b1b = ar.alloc([128, 1024], F32)
        wr = ar.alloc([128, 8, 256], F32)
        rbias = ar.alloc([128, 256], F32)
        wshgu = ar.alloc([128, 8, 512], BF16)
        wshd = ar.alloc([128, 2, 1024], BF16)
        posbase = ar.alloc([128, 256], F32)
        zt = ar.alloc([128, 1024], U32)
        yt = [ar.alloc([128, 1024], BF16) for _ in range(2)]
        yT = [ar.alloc([128, 8, 128], BF16) for _ in range(2)]
        xt = [ar.alloc([128, 1024], F32) for _ in range(2)]
        hh = [ar.alloc([128, 1024], F32) for _ in range(2)]
        h2 = [ar.alloc([128, 1024], F32) for _ in range(2)]
        h2T = [ar.alloc([128, 8, 128], BF16) for _ in range(2)]
        h2Tb = [ar.alloc([128, 8, 128], BF16) for _ in range(2)]
        h2hi = [ar.alloc([128, 1024], BF16) for _ in range(2)]
        h2lo = [ar.alloc([128, 1024], BF16) for _ in range(2)]
        wrh = ar.alloc([128, 8, 256], BF16); wrl = ar.alloc([128, 8, 256], BF16)
        ysh = [ar.alloc([128, 1024], F32) for _ in range(2)]
        stt = [ar.alloc([128, 2, 6], F32) for _ in range(2)]
        stt2 = [ar.alloc([128, 2, 6], F32) for _ in range(2)]
        mv2 = [ar.alloc([128, 8], F32) for _ in range(2)]
        Bmv2 = P.bufs(2)
        mv = [ar.alloc([128, 8], F32) for _ in range(2)]
        sc0 = ar.alloc([128, 256], F32); scores = ar.alloc([128, 256], F32); biased = ar.alloc([128, 256], F32)
        mb = ar.alloc([128, 256], F32); sel = ar.alloc([128, 256], BF16); wsel = ar.alloc([128, 256], F32)
        G = ar.alloc([128, 256], F32); pos = ar.alloc([128, 256], F32); junk = ar.alloc([128, 256], F32)
        top8g = ar.alloc([128, 8, 8], F32); grp = ar.alloc([128, 8], F32); g8 = ar.alloc([128, 8], F32)
        gmask = ar.alloc([128, 8], F32); v8 = ar.alloc([128, 8], F32); i8 = ar.alloc([128, 8], U32)
        i8f = ar.alloc([128, 8], F32); psel = ar.alloc([128, 8], F32); gsel = ar.alloc([128, 8], F32)
        destf = ar.alloc([128, 8], F32); tmpc = ar.alloc([128, 8], F32); sumw = ar.alloc([128, 2], F32)
        info = [ar.alloc([128, 8, 4], U32) for _ in range(2)]
        dslot = [ar.alloc([128, 8], U32) for _ in range(2)]
        tfl = ar.alloc([128, 8], F32); ppos = ar.alloc([128, 8], F32)
        Bds = P.bufs(2)
        Binfo0 = P.bufs(2)
        sA = ar.alloc([128, 256], F32); aT = ar.alloc([128, 256], BF16)
        Bw = P.buf(); Bpb = P.buf(); Bz = P.buf()
        Byt = P.bufs(2); ByT = P.bufs(2); Bxt = P.bufs(2); Bhh = P.bufs(2); Bh2 = P.bufs(2); Bh2T = P.bufs(2); Bh2Tb = P.bufs(2)
        Bysh = P.bufs(2); Bmv = P.bufs(2); Br = P.buf(); Binfo = P.bufs(2); BsA = P.buf(); BaT = P.buf()
        for c in range(8):
            P.dma("pool", lambda e, c=c: e.dma_start(out=wout[:, c, :], in_=w_out[c * 128:(c + 1) * 128, :]), writes=[Bw])
        bcast_load(g0b, ln_in_g, Bw); bcast_load(b0b, ln_in_b, Bw); bcast_load(g1b, ln1_g, Bw); bcast_load(b1b, ln1_b, Bw)
        bcast_load(rbias, router_bias, Bw)
        P.dma("sp", lambda e: e.dma_start(out=wr, in_=w_router.rearrange("(c p) e -> p c e", p=128)), writes=[Bw])
        P.dma("pool", lambda e: e.dma_start(out=wshgu[:, :, 0:256], in_=w_sh_gate.rearrange("(c p) f -> p c f", p=128)), writes=[Bw])
        P.dma("pool", lambda e: e.dma_start(out=wshgu[:, :, 256:512], in_=w_sh_up.rearrange("(c p) f -> p c f", p=128)), writes=[Bw])
        P.dma("pool", lambda e: e.dma_start(out=wshd, in_=w_sh_down.rearrange("(c p) d -> p c d", p=128)), writes=[Bw])
        P.dve(lambda e: e.memset(posbase, 0.0), writes=[Bpb])
        for i in range(2):
            P.dve(lambda e, i=i: e.memset(info[i].bitcast(F32), 0.0), writes=[Binfo[i]])
        P.dve(lambda e: e.tensor_copy(out=wrh, in_=wr), reads=[Bw], writes=[Bw])
        P.dve(lambda e: e.tensor_tensor(out=wrl, in0=wr, in1=wrh, op=ALU.subtract), reads=[Bw], writes=[Bw])
        Bhl = P.bufs(2)
        ab0row = ar.alloc([1, 1024], F32); onesrow = ar.alloc([1, 128], F32)
        P.dma("sp", lambda e: e.dma_start(out=ab0row, in_=ln_in_b.rearrange("(o n) -> o n", o=1)), writes=[Bw])
        P.dve(lambda e: e.tensor_scalar(out=ab0row, in0=ab0row, scalar1=ALPHA, scalar2=None, op0=ALU.mult), reads=[Bw], writes=[Bw])
        P.dve(lambda e: e.tensor_scalar(out=g0b, in0=g0b, scalar1=ALPHA, scalar2=None, op0=ALU.mult), reads=[Bw], writes=[Bw])
        P.dve(lambda e: e.memset(onesrow, 1.0), writes=[Bw])
        P.dma("sp", lambda e: e.dma_start(out=slot_s.rearrange("(p n) w -> p (n w)", p=128), in_=slot_init_d), writes=[Bz])
        def part1a(g):
            pb = g % 2
            lb = 4 + g % 2
            rows = slice(g * 128, (g + 1) * 128)
            P.stage = 1
            P.dma("sp", lambda e, pb=pb, rows=rows: e.dma_start(out=yt[pb], in_=y_s[rows, :]), writes=[Byt[pb]])
            P.dma("sp", lambda e, pb=pb, rows=rows: e.dma_start(out=xt[pb], in_=x[rows, :]), writes=[Bxt[pb]])
            for c in range(8):
                P.pe(lambda e, c=c, pb=pb: e.transpose(out=psb(0)[:, c * 128:(c + 1) * 128], in_=yt[pb][:, c * 128:(c + 1) * 128], identity=ident_bf),
                     reads=[Byt[pb], Bcst], writes=[Bps[0]])
            P.act(lambda e, pb=pb: e.activation(out=yT[pb].rearrange("p a b -> p (a b)"), in_=psb(0)[:, :], func=AF.Copy),
                  reads=[Bps[0]], writes=[ByT[pb]])
            for hf in range(2):
                P.pe(lambda e, hf=hf: e.matmul(ps[1 + hf][:, :], lhsT=onesrow, rhs=ab0row[:, hf * 512:(hf + 1) * 512], start=True, stop=False,
                                               skip_group_check=True), reads=[Bw], writes=[Bps[1 + hf]])
                for c in range(8):
                    P.pe(lambda e, c=c, pb=pb, hf=hf: e.matmul(ps[1 + hf][:, :], lhsT=yT[pb][:, c, :], rhs=wout[:, c, hf * 512:(hf + 1) * 512],
                                                              start=False, stop=(c == 7), skip_group_check=True),
                         reads=[ByT[pb], Bw], writes=[Bps[1 + hf]])
            P.stage = 2
            ln_stats(xt[pb], mv[pb], stt[pb], mv[pb][:, 2:3], Bxt[pb], Bmv[pb])
            P.dve(lambda e, pb=pb: e.scalar_tensor_tensor(out=mv[pb][:, 3:4], in0=mv[pb][:, 0:1], scalar=-1.0, in1=mv[pb][:, 2:3],
                                                          op0=ALU.mult, op1=ALU.mult), reads=[Bmv[pb]], writes=[Bmv[pb]])
            P.act(lambda e, pb=pb: e.activation(out=hh[pb], in_=xt[pb], func=AF.Identity, scale=mv[pb][:, 2:3], bias=mv[pb][:, 3:4]),
                  reads=[Bxt[pb], Bmv[pb]], writes=[Bhh[pb]])
            P.dve(lambda e, pb=pb: e.tensor_tensor(out=hh[pb], in0=hh[pb], in1=g0b, op=ALU.mult), reads=[Bhh[pb], Bw], writes=[Bhh[pb]])
            for hf in range(2):
                P.dve(lambda e, pb=pb, hf=hf: e.tensor_tensor(out=hh[pb][:, hf * 512:(hf + 1) * 512], in0=hh[pb][:, hf * 512:(hf + 1) * 512],
                                                              in1=ps[1 + hf][:, :], op=ALU.add),
                      reads=[Bhh[pb], Bps[1 + hf]], writes=[Bhh[pb]])

        def part1b(g):
            pb = g % 2
            lb = 4 + g % 2
            rows = slice(g * 128, (g + 1) * 128)
            ln_stats(hh[pb], mv2[pb][:, 4:8], stt2[pb], mv2[pb][:, 6:7], Bhh[pb], Bmv2[pb])
            P.dve(lambda e, pb=pb: e.scalar_tensor_tensor(out=mv2[pb][:, 7:8], in0=mv2[pb][:, 4:5], scalar=-1.0, in1=mv2[pb][:, 6:7],
                                                          op0=ALU.mult, op1=ALU.mult), reads=[Bmv2[pb]], writes=[Bmv2[pb]])
            P.act(lambda e, pb=pb: e.activation(out=h2[pb], in_=hh[pb], func=AF.Identity, scale=mv2[pb][:, 6:7], bias=mv2[pb][:, 7:8]),
                  reads=[Bhh[pb], Bmv2[pb]], writes=[Bh2[pb]])
            P.dve(lambda e, pb=pb: e.tensor_tensor(out=h2[pb], in0=h2[pb], in1=g1b, op=ALU.mult), reads=[Bh2[pb], Bw], writes=[Bh2[pb]])
            P.dve(lambda e, pb=pb: e.tensor_tensor(out=h2[pb], in0=h2[pb], in1=b1b, op=ALU.add), reads=[Bh2[pb], Bw], writes=[Bh2[pb]])
            P.dma("sp", lambda e, pb=pb, rows=rows: e.dma_start(out=h2_s[rows, :], in_=h2[pb]), reads=[Bh2[pb]])
            P.dma("pool", lambda e, pb=pb, rows=rows: e.dma_start(out=h2b_s[rows, :], in_=h2[pb]), reads=[Bh2[pb]])
            P.stage = 3
            P.act(lambda e, pb=pb: e.activation(out=h2hi[pb], in_=h2[pb], func=AF.Copy), reads=[Bh2[pb]], writes=[Bhl[pb]])
            P.dve(lambda e, pb=pb: e.tensor_tensor(out=h2lo[pb], in0=h2[pb], in1=h2hi[pb], op=ALU.subtract), reads=[Bh2[pb], Bhl[pb]], writes=[Bhl[pb]])
            for c in range(8):
                P.pe(lambda e, c=c, pb=pb: e.transpose(out=psb(3)[:, c * 128:(c + 1) * 128], in_=h2hi[pb][:, c * 128:(c + 1) * 128], identity=ident_bf),
                     reads=[Bhl[pb], Bcst], writes=[Bps[3]])
            P.act(lambda e, pb=pb: e.activation(out=h2Tb[pb].rearrange("p a b -> p (a b)"), in_=psb(3)[:, :], func=AF.Copy),
                  reads=[Bps[3]], writes=[Bh2Tb[pb]])
            for c in range(8):
                P.pe(lambda e, c=c, pb=pb: e.transpose(out=psb(3)[:, c * 128:(c + 1) * 128], in_=h2lo[pb][:, c * 128:(c + 1) * 128], identity=ident_bf),
                     reads=[Bhl[pb], Bcst], writes=[Bps[3]])
            P.act(lambda e, pb=pb: e.activation(out=h2T[pb].rearrange("p a b -> p (a b)"), in_=psb(3)[:, :], func=AF.Copy),
                  reads=[Bps[3]], writes=[Bh2T[pb]])
            combos = [(h2Tb, wrh), (h2T, wrh), (h2Tb, wrl)]
            for ci, (lt_, wt_) in enumerate(combos):
                for c in range(8):
                    P.pe(lambda e, c=c, pb=pb, lt_=lt_, wt_=wt_, ci=ci, lb=lb: e.matmul(ps[lb][:, 0:256], lhsT=lt_[pb][:, c, :], rhs=wt_[:, c, :],
                                                                                start=(ci == 0 and c == 0), stop=(ci == 2 and c == 7),
                                                                                skip_group_check=True),
                         reads=[Bh2T[pb], Bh2Tb[pb], Bw], writes=[Bps[lb]])

        def part2a(g):
            pb = g % 2
            lb = 4 + g % 2
            rows = slice(g * 128, (g + 1) * 128)
            P.stage = 5
            for fc in range(4):
                for c in range(8):
                    P.pe(lambda e, c=c, fc=fc, pb=pb: e.matmul(ps[7][:, fc * 128:(fc + 1) * 128], lhsT=wshgu[:, c, fc * 128:(fc + 1) * 128],
                                                              rhs=h2Tb[pb][:, c, :], start=(c == 0), stop=(c == 7), skip_group_check=True),
                         reads=[Bh2Tb[pb], Bw], writes=[Bps[7]])
            P.act(lambda e: e.activation(out=sA, in_=ps[7][:, 0:256], func=AF.Exp, scale=-1.0), reads=[Bps[7]], writes=[BsA])
            P.dve(lambda e: e.tensor_scalar(out=sA, in0=sA, scalar1=1.0, scalar2=None, op0=ALU.add), reads=[BsA], writes=[BsA])
            P.dve(lambda e: e.reciprocal(out=sA, in_=sA), reads=[BsA], writes=[BsA])
            P.dve(lambda e: e.tensor_tensor(out=sA, in0=sA, in1=ps[7][:, 0:256], op=ALU.mult), reads=[BsA, Bps[7]], writes=[BsA])
            P.dve(lambda e: e.tensor_tensor(out=aT, in0=sA, in1=ps[7][:, 256:512], op=ALU.mult), reads=[BsA, Bps[7]], writes=[BaT])
            for hf in range(2):
                for f2 in range(2):
                    P.pe(lambda e, hf=hf, f2=f2: e.matmul(ps[7][:, :], lhsT=aT[:, f2 * 128:(f2 + 1) * 128], rhs=wshd[:, f2, hf * 512:(hf + 1) * 512],
                                                         start=(f2 == 0), stop=(f2 == 1)), reads=[BaT, Bw], writes=[Bps[7]])
                P.act(lambda e, hf=hf, pb=pb: e.activation(out=ysh[pb][:, hf * 512:(hf + 1) * 512], in_=ps[7][:, :], func=AF.Copy),
                      reads=[Bps[7]], writes=[Bysh[pb]])
            P.dma("sp", lambda e, pb=pb, rows=rows: e.dma_start(out=ysh_s[rows, :], in_=ysh[pb]), reads=[Bysh[pb]])

        def part2b(g):
            pb = g % 2
            lb = 4 + g % 2
            rows = slice(g * 128, (g + 1) * 128)
            P.stage = 4
            R = [Br]
            P.act(lambda e, lb=lb: e.activation(out=sc0, in_=ps[lb][:, 0:256], func=AF.Exp, scale=-1.0), reads=[Bps[lb]], writes=R)
            P.dve(lambda e: e.tensor_scalar(out=sc0, in0=sc0, scalar1=1.0, scalar2=None, op0=ALU.add), reads=R, writes=R)
            P.dve(lambda e: e.reciprocal(out=scores, in_=sc0), reads=R, writes=R)
            P.dve(lambda e: e.tensor_tensor(out=biased, in0=scores, in1=rbias, op=ALU.add), reads=R + [Bw], writes=R)
            for gi in range(8):
                P.dve(lambda e, gi=gi: e.max(out=top8g[:, gi, :], in_=biased[:, gi * 32:(gi + 1) * 32]), reads=R, writes=R)
            P.dve(lambda e: e.tensor_tensor(out=grp, in0=top8g[:, :, 0], in1=top8g[:, :, 1], op=ALU.add), reads=R, writes=R)
            P.dve(lambda e: e.max(out=g8, in_=grp), reads=R, writes=R)
            P.dve(lambda e: e.tensor_scalar(out=gmask, in0=grp, scalar1=g8[:, 3:4], scalar2=None, op0=ALU.is_ge), reads=R, writes=R)
            P.dve(lambda e: e.scalar_tensor_tensor(out=mb.rearrange("p (g k) -> p g k", k=32), in0=biased.rearrange("p (g k) -> p g k", k=32),
                                                   scalar=1.0, in1=gmask.unsqueeze(2).to_broadcast([128, 8, 32]), op0=ALU.add, op1=ALU.mult),
                  reads=R, writes=R)
            P.dve(lambda e: e.max(out=v8, in_=mb), reads=R, writes=R)
            P.dve(lambda e: e.tensor_scalar(out=sel, in0=mb, scalar1=v8[:, 7:8], scalar2=None, op0=ALU.is_ge), reads=R, writes=R)
            P.dve(lambda e: e.scalar_tensor_tensor(out=wsel, in0=scores, scalar=1.0, in1=sel, op0=ALU.mult, op1=ALU.mult, accum_out=sumw[:, 0:1]),
                  reads=R, writes=R)
            P.dve(lambda e: e.reciprocal(out=sumw[:, 1:2], in_=sumw[:, 0:1]), reads=R, writes=R)
            P.dve(lambda e: e.max(out=g8, in_=wsel), reads=R, writes=R)
            P.dve(lambda e: e.max_index(out=i8, in_max=g8, in_values=wsel), reads=R, writes=R)
            P.dve(lambda e: e.tensor_scalar(out=gsel, in0=g8, scalar1=sumw[:, 1:2], scalar2=2.5, op0=ALU.mult, op1=ALU.mult), reads=R, writes=R)
            P.pe(lambda e: e.matmul(ps[6][:, 0:256], lhsT=lt_bf, rhs=sel, start=True, stop=True, skip_group_check=True),
                 reads=R + [Bcst], writes=[Bps[6]])
            P.pe(lambda e: e.matmul(ps[6][:, 256:512], lhsT=ones_bf, rhs=sel, start=True, stop=True, skip_group_check=True), reads=R + [Bcst], writes=[Bps[6]])
            P.dve(lambda e: e.tensor_tensor(out=pos, in0=ps[6][:, 0:256], in1=posbase, op=ALU.add), reads=[Bps[6], Bpb], writes=R)
            P.dve(lambda e: e.tensor_tensor(out=posbase, in0=ps[6][:, 256:512], in1=posbase, op=ALU.add), reads=[Bps[6], Bpb], writes=[Bpb])
            P.dve(lambda e: e.tensor_copy(out=i8f, in_=i8), reads=R, writes=R)
            for k in range(8):
                P.dve(lambda e, k=k: e.scalar_tensor_tensor(out=junk, in0=iota_f, scalar=i8f[:, k:k + 1], in1=pos, op0=ALU.is_equal, op1=ALU.mult,
                                                            accum_out=psel[:, k:k + 1]), reads=R + [Bcst], writes=R)
            P.dve(lambda e: e.tensor_scalar(out=tmpc, in0=psel, scalar1=float(CAP - 1), scalar2=None, op0=ALU.min), reads=R, writes=R)
            P.dve(lambda e: e.scalar_tensor_tensor(out=destf, in0=i8f, scalar=float(CAP), in1=tmpc, op0=ALU.mult, op1=ALU.add), reads=R, writes=R)
            P.dve(lambda e, g=g: e.tensor_copy(out=destall[:, g, :], in_=destf), reads=R, writes=[Bdest[g]])
            P.dve(lambda e: e.tensor_scalar(out=tfl, in0=tmpc, scalar1=128.0, scalar2=None, op0=ALU.is_ge), reads=R, writes=R)
            P.dve(lambda e: e.scalar_tensor_tensor(out=ppos, in0=tfl, scalar=-128.0, in1=tmpc, op0=ALU.mult, op1=ALU.add), reads=R, writes=R)
            P.dve(lambda e: e.scalar_tensor_tensor(out=tfl, in0=i8f, scalar=2.0, in1=tfl, op0=ALU.mult, op1=ALU.add), reads=R, writes=R)
            P.dve(lambda e: e.scalar_tensor_tensor(out=ppos, in0=ppos, scalar=512.0, in1=tfl, op0=ALU.mult, op1=ALU.add), reads=R, writes=R)
            P.dve(lambda e, pb=pb: e.tensor_copy(out=dslot[pb], in_=ppos), reads=R, writes=[Bds[pb]])
            P.dve(lambda e, g=g, pb=pb: e.tensor_copy(out=info[pb][:, :, 0], in_=tok_f[:, g:g + 1].to_broadcast([128, 8])),
                  reads=[Bcst], writes=[Binfo[pb]])
            P.dve(lambda e, pb=pb: e.tensor_copy(out=info[pb].bitcast(F32)[:, :, 1], in_=gsel), reads=R, writes=[Binfo[pb]])
            P.dve(lambda e, pb=pb: e.tensor_copy(out=info[pb][:, :, 2], in_=destf), reads=R, writes=[Binfo[pb]])
            for k in range(0 if NOSCAT else 8):
                P.dma("pool", lambda e, g=g, k=k, pb=pb: e.indirect_dma_start(
                    out=slot_s, out_offset=bass.IndirectOffsetOnAxis(ap=dslot[pb][:, k:k + 1], axis=0), in_=info[pb][:, k, :], in_offset=None),
                    reads=[Bds[pb], Binfo[pb], Bz])

        def cap(fn, g):
            P.cap_begin()
            if g < NT:
                fn(g)
            return P.cap_end()

        part1a(0)
        P.interleave(cap(part1b, 0), cap(part1a, 1))
        for g in range(NT):
            P.interleave(cap(part2b, g), cap(part2a, g), cap(part1b, g + 1), cap(part1a, g + 2))
        P.stage = 0
        if debug:
            P.dma("sp", lambda e: e.dma_start(out=dbg_dest, in_=destall.rearrange("p a b -> p (a b)")), reads=Bdest)
        P.barrier(bscr)
        ar.release(gmark)

    def phase_E():
        NW = 4
        wg = [ar.alloc([128, 8, 256], BF16) for _ in range(NW)]
        wu = [ar.alloc([128, 8, 256], BF16) for _ in range(NW)]
        wd = [ar.alloc([128, 2, 1024], BF16) for _ in range(NW)]
        siall = ar.alloc([128, NE * 2, 4], U32)
        xg = [ar.alloc([128, 1024], BF16) for _ in range(6)]
        xgT = [ar.alloc([128, 8, 256], BF16) for _ in range(2)]
        sl = [ar.alloc([128, 512], BF16) for _ in range(2)]
        aT = [ar.alloc([128, 2, 256], BF16) for _ in range(2)]
        yst = [ar.alloc([128, 1024], BF16) for _ in range(4)]
        Bwg = P.bufs(NW); Bwu = P.bufs(NW); Bwd = P.bufs(NW); Bsi = P.buf(); Bxg = P.bufs(6); BxgT = P.bufs(2)
        Bsl = P.bufs(2); BaT = P.bufs(2); Byst = P.bufs(4)
        si_f = siall.bitcast(F32)
        P.dma("sp", lambda e: e.dma_start(out=siall.rearrange("p r w -> p (r w)"), in_=slot_s.rearrange("(p r) w -> p (r w)", p=128)), writes=[Bsi])

        for i in range(6):
            P.dve(lambda e, i=i: e.memset(xg[i], 0.0), writes=[Bxg[i]])

        def W(ex):
            wb = ex % NW
            P.dma("pool", lambda e, ex=ex, wb=wb: e.dma_start(out=wg[wb].rearrange("p c f -> p (c f)"),
                                                             in_=w_exp_gate[ex].rearrange("(p c) f -> p (c f)", c=8)), writes=[Bwg[wb]])
            P.dma("pool", lambda e, ex=ex, wb=wb: e.dma_start(out=wu[wb].rearrange("p c f -> p (c f)"),
                                                             in_=w_exp_up[ex].rearrange("(p c) f -> p (c f)", c=8)), writes=[Bwu[wb]])
            P.dma("pool", lambda e, ex=ex, wb=wb: e.dma_start(out=wd[wb].rearrange("p c d -> p (c d)"),
                                                             in_=w_exp_down[ex].rearrange("(p c) d -> p (c d)", c=2)), writes=[Bwd[wb]])

        def Gt(ex):
            for t in range(2):
                xi = 2 * (ex % 3) + t
                P.dma("pool", lambda e, xi=xi, ex=ex, t=t: e.indirect_dma_start(
                    out=xg[xi], out_offset=None, in_=h2b_s, in_offset=bass.IndirectOffsetOnAxis(ap=siall[:, 2 * ex + t, 0:1], axis=0),
                    bounds_check=_breg(e, S - 1), oob_is_err=False), reads=[Bsi], writes=[Bxg[xi]])

        def T(ex):
            xb = ex % 2
            for t in range(2):
                xi = 2 * (ex % 3) + t
                for c in range(8):
                    P.pe(lambda e, c=c, xi=xi, t=t: e.transpose(out=psb(t)[:, c * 128:(c + 1) * 128], in_=xg[xi][:, c::8],
                                                                identity=ident_bf), reads=[Bxg[xi], Bcst], writes=[Bps[t]])
                src_v = psb(t)[:, :].rearrange("p (a b) -> p a b", a=8)
                if t == 0:
                    P.act(lambda e, xb=xb, t=t, src_v=src_v: e.activation(out=xgT[xb][:, :, t * 128:(t + 1) * 128], in_=src_v, func=AF.Copy),
                          reads=[Bps[t]], writes=[BxgT[xb]])
                else:
                    P.dve(lambda e, xb=xb, t=t, src_v=src_v: e.tensor_copy(out=xgT[xb][:, :, t * 128:(t + 1) * 128], in_=src_v),
                          reads=[Bps[t]], writes=[BxgT[xb]])

        def H(ex):
            wb = ex % NW
            xb = ex % 2
            for fc in range(4):
                bank = 2 + fc // 2
                w_, Bw_ = (wg, Bwg) if fc < 2 else (wu, Bwu)
                f0 = (fc % 2) * 128
                for c in range(8):
                    P.pe(lambda e, c=c, bank=bank, w_=w_, f0=f0, fc=fc, wb=wb, xb=xb: e.matmul(
                        ps[bank][:, (fc % 2) * 256:(fc % 2 + 1) * 256], lhsT=w_[wb][:, c, (fc % 2)::2], rhs=xgT[xb][:, c, :],
                        start=(c == 0), stop=(c == 7), skip_group_check=True), reads=[Bw_[wb], BxgT[xb]], writes=[Bps[bank]])
            P.act(lambda e, xb=xb: e.activation(out=sl[xb], in_=ps[2][:, :], func=AF.Silu), reads=[Bps[2]], writes=[Bsl[xb]])
            P.dve(lambda e, xb=xb: e.tensor_tensor(out=aT[xb].rearrange("p a b -> p (a b)"), in0=sl[xb], in1=ps[3][:, :], op=ALU.mult),
                  reads=[Bsl[xb], Bps[3]], writes=[BaT[xb]])

        def Y(ex):
            wb = ex % NW
            xb = ex % 2
            for t in range(2):
                yi = 2 * xb + t
                gate_ap = si_f[:, 2 * ex + t, 1:2]
                for hf in range(2):
                    bank = 4 + 2 * t + hf
                    for f2 in range(2):
                        P.pe(lambda e, bank=bank, f2=f2, t=t, hf=hf, xb=xb, wb=wb: e.matmul(
                            ps[bank][:, :], lhsT=aT[xb][:, f2, t * 128:(t + 1) * 128], rhs=wd[wb][:, f2, hf * 512:(hf + 1) * 512],
                            start=(f2 == 0), stop=(f2 == 1)), reads=[BaT[xb], Bwd[wb]], writes=[Bps[bank]])
                    if hf == 0:
                        P.act(lambda e, bank=bank, yi=yi, gate_ap=gate_ap: e.activation(out=yst[yi][:, 0:512], in_=ps[bank][:, :], func=AF.Copy,
                                                                                        scale=gate_ap), reads=[Bps[bank], Bsi], writes=[Byst[yi]])
                    else:
                        P.dve(lambda e, bank=bank, yi=yi, gate_ap=gate_ap: e.tensor_scalar(out=yst[yi][:, 512:1024], in0=ps[bank][:, :],
                                                                                           scalar1=gate_ap, scalar2=None, op0=ALU.mult),
                              reads=[Bps[bank], Bsi], writes=[Byst[yi]])
                P.dma("pool", lambda e, yi=yi, ex=ex, t=t: e.indirect_dma_start(
                    out=Y_s, out_offset=bass.IndirectOffsetOnAxis(ap=siall[:, 2 * ex + t, 2:3], axis=0), in_=yst[yi], in_offset=None,
                    bounds_check=_breg(e, NE * CAP - 1), oob_is_err=False), reads=[Byst[yi], Bsi])

        W(0); W(1); Gt(0); Gt(1)
        T(0)
        for ex in range(NE):
            if ex + 2 < NE:
                W(ex + 2)
                Gt(ex + 2)
            H(ex)
            if ex + 1 < NE:
                T(ex + 1)
            Y(ex)
        P.barrier(bscr)
        ar.release(gmark)

    def phase_F():
        g2b = ar.alloc([128, 1024], F32); b2b = ar.alloc([128, 1024], F32)
        NB = 4
        h2t = [ar.alloc([128, 1024], F32) for _ in range(NB)]
        ysht = [ar.alloc([128, 1024], F32) for _ in range(NB)]
        Yk = [[ar.alloc([128, 1024], BF16) for _ in range(8)] for _ in range(NB)]
        stt = [ar.alloc([128, 2, 6], F32) for _ in range(NB)]
        mv = [ar.alloc([128, 4], F32) for _ in range(NB)]
        Bw = P.buf(); Bh = P.bufs(NB); Bys = P.bufs(NB); BY = [P.bufs(8) for _ in range(NB)]; Bmv = P.bufs(NB)
        bcast_load(g2b, ln2_g, Bw); bcast_load(b2b, ln2_b, Bw)
        def tile_F(g):
            pb = g % NB
            rows = slice(g * 128, (g + 1) * 128)
            bk = 2 * (g % 4)
            P.dma("pool", lambda e, pb=pb, rows=rows: e.dma_start(out=h2t[pb], in_=h2_s[rows, :]), writes=[Bh[pb]])
            P.dma("pool", lambda e, pb=pb, rows=rows: e.dma_start(out=ysht[pb], in_=ysh_s[rows, :]), writes=[Bys[pb]])
            for k in range(8):
                P.dma("pool", lambda e, pb=pb, k=k, g=g: e.indirect_dma_start(
                    out=Yk[pb][k], out_offset=None, in_=Y_s, in_offset=bass.IndirectOffsetOnAxis(ap=destall[:, g, k:k + 1], axis=0)),
                    reads=[Bdest[g]], writes=[BY[pb][k]])
            for hf in range(2):
                for k in range(8):
                    P.pe(lambda e, pb=pb, k=k, hf=hf, bk=bk: e.matmul(ps[bk + hf][:, :], lhsT=ident_bf, rhs=Yk[pb][k][:, hf * 512:(hf + 1) * 512],
                                                                     start=(k == 0), stop=(k == 7)), reads=[BY[pb][k], Bcst], writes=[Bps[bk + hf]])
            r = h2t[pb]
            P.dve(lambda e, r=r, pb=pb: e.scalar_tensor_tensor(out=r, in0=r, scalar=ALPHA, in1=ysht[pb], op0=ALU.mult, op1=ALU.add),
                  reads=[Bh[pb], Bys[pb]], writes=[Bh[pb]])
            for hf in range(2):
                P.dve(lambda e, r=r, hf=hf, bk=bk: e.tensor_tensor(out=r[:, hf * 512:(hf + 1) * 512], in0=r[:, hf * 512:(hf + 1) * 512],
                                                                  in1=ps[bk + hf][:, :], op=ALU.add), reads=[Bh[pb], Bps[bk + hf]], writes=[Bh[pb]])
            ln_stats(r, mv[pb], stt[pb], mv[pb][:, 2:3], Bh[pb], Bmv[pb])
            P.dve(lambda e, pb=pb: e.scalar_tensor_tensor(out=mv[pb][:, 3:4], in0=mv[pb][:, 0:1], scalar=-1.0, in1=mv[pb][:, 2:3],
                                                          op0=ALU.mult, op1=ALU.mult), reads=[Bmv[pb]], writes=[Bmv[pb]])
            P.act(lambda e, r=r, pb=pb: e.activation(out=r, in_=r, func=AF.Identity, scale=mv[pb][:, 2:3], bias=mv[pb][:, 3:4]),
                  reads=[Bh[pb], Bmv[pb]], writes=[Bh[pb]])
            P.dve(lambda e, r=r: e.tensor_tensor(out=r, in0=r, in1=g2b, op=ALU.mult), reads=[Bh[pb], Bw], writes=[Bh[pb]])
            P.dve(lambda e, r=r: e.tensor_tensor(out=r, in0=r, in1=b2b, op=ALU.add), reads=[Bh[pb], Bw], writes=[Bh[pb]])
            def store_fn(r=r, rows=rows):
                fn_ = lambda e: e.dma_start(out=out[rows, :], in_=r)
                fn_.is_out = True
                return fn_
            P.dma("sp", store_fn(), reads=[Bh[pb]])

        for g2 in range(0, NT, 2):
            P.cap_begin(); tile_F(g2); la = P.cap_end()
            P.cap_begin(); tile_F(g2 + 1); lb = P.cap_end()
            P.interleave(la, lb)
        outs.extend(o for o in P.ops if o.dma and getattr(o.fn, "is_out", False))

    phase_A()
    if stop_after >= "B":
        phase_B()
    if stop_after >= "C":
        phase_C()
    if stop_after >= "D":
        phase_D()
    if stop_after >= "E":
        phase_E()
    if stop_after >= "F":
        phase_F()
    P.emit(final_wait_ops=outs)
    return nc, P


def _consts():
    c = np.zeros((128, C_END), np.float32)
    p = np.arange(128)[:, None]
    j = np.arange(128)[None, :]
    c[:, C_ID:C_ID + 128] = (p == j)
    c[:, C_LE:C_LE + 128] = (p <= j)
    c[:, C_LT:C_LT + 128] = (p < j)
    c[:, C_IOTA:C_IOTA + 256] = np.arange(256)[None, :]
    c[:, C_TOK:C_TOK + 32] = np.arange(32)[None, :] * 128 + p
    return c


def _slot_init():
    row = np.array([S + 4095, 0, NE * CAP + 4095, 0], np.uint32)
    return np.ascontiguousarray(np.tile(row, (128, 512)))


def make_in_map(inputs, b):
    g = np.asarray(inputs["ln_in_g"], np.float32)
    bb = np.asarray(inputs["ln_in_b"], np.float32)
    lnfm = np.concatenate([g.reshape(8, 128).T, bb.reshape(8, 128).T], axis=1)
    m = {"x": np.ascontiguousarray(inputs["x"][b]), "cst": _consts(), "slot_init": _slot_init(), "lnfm": np.ascontiguousarray(lnfm),
         "ln_in_g": g, "ln_in_b": bb}
    for k in IN_NAMES:
        if k in m:
            continue
        m[k] = np.ascontiguousarray(np.asarray(inputs[k])[0])
    return m


_CACHE = {}


def kernel(**inputs):
    if "nc" not in _CACHE:
        _CACHE["nc"] = build()[0]
    nc = _CACHE["nc"]
    in_maps = [make_in_map(inputs, b) for b in range(8)]
    res = run_bass_kernel_spmd(nc, in_maps, core_ids=list(range(8)))
    return np.stack([np.asarray(r["out"], np.float32) for r in res.results], axis=0)
```

```python
import contextlib
import os
import numpy as np
import concourse.bass as bass
import concourse.mybir as mybir
from concourse.bass_utils import run_bass_kernel_spmd

F32 = mybir.dt.float32
BF16 = mybir.dt.bfloat16
U32 = mybir.dt.uint32
U8 = mybir.dt.uint8
AF = mybir.ActivationFunctionType
ALU = mybir.AluOpType
AX = mybir.AxisListType

SEM_WRAP = 30000
NDMASEM = 24
S = 4096
D = 1024
NT = 32
NE = 256
CAP = 256
EPS = 1e-5
ALPHA = 2.0 ** 0.25
ARENA = 184 * 1024


class Buf:
    __slots__ = ("name", "w", "r")

    def __init__(self, name=""):
        self.name = name
        self.w = None
        self.r = []


class Op:
    __slots__ = ("eng", "fn", "dma", "deps", "sig", "seq", "dsem", "dval", "flow", "cidx")

    def __init__(self, eng, fn, dma):
        self.eng = eng
        self.fn = fn
        self.dma = dma
        self.deps = []
        self.sig = False
        self.seq = None
        self.dsem = None
        self.dval = None
        self.flow = None


class Prog:
    ENGS = ("pe", "act", "dve", "pool", "sp")

    def __init__(self, nc):
        self.nc = nc
        self.ops = []
        self.stack = contextlib.ExitStack()
        self.barrier_op = None
        self.last = {}
        self.dmas_since = []

    def buf(self):
        return Buf()

    def bufs(self, n):
        return [Buf() for _ in range(n)]

    stage = 0
    cut = int(os.environ.get("KC_CUT", "99"))

    capture = None
    DIST = 1 << 30

    def cap_begin(self):
        self.capture = []

    def cap_end(self):
        c, self.capture = self.capture, None
        return c

    def play(self, lst, k=None):
        n = len(lst) if k is None else min(k, len(lst))
        for _ in range(n):
            a = lst.pop(0)
            self.add(*a)

    def interleave(self, *lists):
        lists = [l for l in lists if l]
        idx = [0] * len(lists)
        while True:
            best, bk = None, -1
            for k, l in enumerate(lists):
                if idx[k] < len(l):
                    frac = idx[k] / len(l)
                    if best is None or frac < best:
                        best, bk = frac, k
            if bk < 0:
                break
            self.add(*lists[bk][idx[bk]])
            idx[bk] += 1

    def add(self, eng, fn, reads=(), writes=(), dma=False):
        if self.capture is not None:
            self.capture.append((eng, fn, tuple(reads), tuple(writes), dma))
            return None
        op = Op(eng, fn, dma)
        if self.stage > self.cut:
            return op
        seen = {}
        for b in reads:
            if b.w is not None:
                seen[id(b.w)] = (b.w, True)
        for b in writes:
            if b.w is not None and id(b.w) not in seen:
                seen[id(b.w)] = (b.w, False)
            for r in b.r:
                if id(r) not in seen:
                    seen[id(r)] = (r, False)
        seen.pop(id(op), None)
        op.deps = list(seen.values())
        if self.barrier_op is not None:
            op.deps.append((self.barrier_op, True))
        for b in writes:
            b.w = op
            b.r = []
        for b in reads:
            b.r.append(op)
        self.ops.append(op)
        if dma:
            self.dmas_since.append(op)
        else:
            self.last[eng] = op
        return op

    def pe(self, fn, reads=(), writes=()):
        return self.add("pe", fn, reads, writes)

    def act(self, fn, reads=(), writes=()):
        return self.add("act", fn, reads, writes)

    def dve(self, fn, reads=(), writes=()):
        return self.add("dve", fn, reads, writes)

    def pool(self, fn, reads=(), writes=()):
        return self.add("pool", fn, reads, writes)

    def dma(self, eng, fn, reads=(), writes=()):
        return self.add(eng, fn, reads, writes, dma=True)

    def barrier(self, scratch):
        deps = [(o, True) for o in self.last.values()] + [(o, True) for o in self.dmas_since]
        op = Op("dve", lambda e: e.memset(scratch, 0.0), False)
        op.deps = deps
        if self.barrier_op is not None:
            op.deps.append((self.barrier_op, True))
        self.ops.append(op)
        self.barrier_op = op
        self.last = {"dve": op}
        self.dmas_since = []
        return op

    def _skip(self, d, op, raw):
        if d.dma or op.dma:
            return False
        if d.eng != op.eng:
            return False
        return d.eng == "pe"

    def emit(self, final_wait_ops=()):
        nc = self.nc
        st = self.stack
        cc = {e: 0 for e in self.ENGS}
        for op in self.ops:
            if not op.dma:
                cc[op.eng] += 1
            op.cidx = cc[op.eng]
        for op in self.ops:
            for d, raw in op.deps:
                if d.dma or self._skip(d, op, raw):
                    continue
                d.sig = True
        cnt = {e: 0 for e in self.ENGS}
        for op in self.ops:
            if not op.dma and op.sig:
                cnt[op.eng] += 1
                op.seq = cnt[op.eng]
        esems = {}
        for e in self.ENGS:
            n = max(1, (cnt[e] + SEM_WRAP - 1) // SEM_WRAP)
            esems[e] = [st.enter_context(nc.semaphore(f"s_{e}{i}")) for i in range(n)]
        dcnt = {}
        dsems = {}
        dlast = {}
        for op in self.ops:
            if op.dma:
                e = op.eng
                if e not in dsems:
                    dsems[e] = [st.enter_context(nc.semaphore(f"d_{e}{i}")) for i in range(NDMASEM)]
                    dlast[e] = [None] * NDMASEM
                    dcnt[e] = 0
                n = dcnt[e]
                dcnt[e] += 1
                k = n % NDMASEM
                op.dsem = dsems[e][k]
                op.dval = 16 * (n // NDMASEM + 1)
                op.flow = dlast[e][k]
                dlast[e][k] = op

        def target(d):
            if d.dma:
                return d.dsem, d.dval
            s = (d.seq - 1) // SEM_WRAP
            return esems[d.eng][s], (d.seq - 1) % SEM_WRAP + 1

        per = {e: [] for e in self.ENGS}
        for op in self.ops:
            per[op.eng].append(op)
        self.stats = {e: len(per[e]) for e in self.ENGS}
        nw = [0]

        def run_engine(ename, eng):
            waited = {}
            for op in per[ename]:
                need = {}
                deps = list(op.deps)
                if op.flow is not None:
                    deps.append((op.flow, True))
                for d, raw in deps:
                    if self._skip(d, op, raw):
                        continue
                    sem, val = target(d)
                    key = sem.num
                    if waited.get(key, 0) >= val:
                        continue
                    if key not in need or need[key][1] < val:
                        need[key] = (sem, val)
                for key, (sem, val) in need.items():
                    waited[key] = val
                    eng.wait_ge(sem, val)
                    nw[0] += 1
                ins = op.fn(eng)
                if op.dma:
                    ins.then_inc(op.dsem, 16)
                elif op.sig:
                    ins.then_inc(target(op)[0], 1)
            if ename == "sp":
                for op in final_wait_ops:
                    sem, val = target(op)
                    eng.wait_ge(sem, val)

        with nc.Block() as block:
            @block.tensor
            def _(e):
                run_engine("pe", e)

            @block.scalar
            def _(e):
                run_engine("act", e)

            @block.vector
            def _(e):
                run_engine("dve", e)

            @block.gpsimd
            def _(e):
                run_engine("pool", e)

            @block.sync
            def _(e):
                run_engine("sp", e)
        self.stats["waits"] = nw[0]
        self.stack.close()


_DS = {F32: 4, BF16: 2, U32: 4, U8: 1}

_BREG = {}


def _breg(e, val):
    k = (id(e), val)
    if k not in _BREG:
        _BREG[k] = e.to_reg(val)
    return _BREG[k]


class Arena:
    def __init__(self, t, size):
        self.t = t
        self.size = size
        self.off = 0
        self.peak = 0

    def alloc(self, shape, dtype):
        n = 1
        for s in shape[1:]:
            n *= s
        nb = n * _DS[dtype]
        off = self.off
        self.off += (nb + 63) // 64 * 64
        self.peak = max(self.peak, self.off)
        assert self.off <= self.size, f"arena overflow {self.off}"
        v = self.t[:, off:off + nb].bitcast(dtype)
        if shape[0] < 128:
            v = v[0:shape[0], :]
        if len(shape) == 3:
            v = v.rearrange("p (a b) -> p a b", a=shape[1])
        elif len(shape) == 4:
            v = v.rearrange("p (a b c) -> p a b c", a=shape[1], b=shape[2])
        return v

    def mark(self):
        return self.off

    def release(self, m):
        self.off = m


C_ID, C_LE, C_LT, C_IOTA, C_TOK, C_END = 0, 128, 256, 384, 640, 672

IN_NAMES = ["x", "cst", "slot_init", "lnfm", "ln_in_g", "ln_in_b", "w_in", "b_fgate", "w_gate_up", "b_gate", "g_gla_norm", "w_out",
            "ln1_g", "ln1_b", "w_router", "router_bias", "w_exp_gate", "w_exp_up", "w_exp_down",
            "w_sh_gate", "w_sh_up", "w_sh_down", "ln2_g", "ln2_b"]


def build(stop_after="F", debug=False):
    nc = bass.Bass("TRN2", target_bir_lowering=False)
    P = Prog(nc)

    def din(name, shape, dt=F32):
        return nc.dram_tensor(name, list(shape), dt, kind="ExternalInput").ap()

    skind = "ExternalOutput" if debug else "Internal"

    def dscr(name, shape, dt):
        return nc.dram_tensor(name, list(shape), dt, kind=skind).ap()

    x = din("x", [S, D])
    cst_d = din("cst", [128, C_END])
    slot_init_d = din("slot_init", [128, 2048], U32)
    lnfm_d = din("lnfm", [128, 16])
    ln_in_g = din("ln_in_g", [D]); ln_in_b = din("ln_in_b", [D])
    w_in = din("w_in", [D, 3096])
    b_fgate = din("b_fgate", [8])
    w_gate_up = din("w_gate_up", [16, 256]); b_gate = din("b_gate", [256])
    g_gla_norm = din("g_gla_norm", [128])
    w_out = din("w_out", [D, D])
    ln1_g = din("ln1_g", [D]); ln1_b = din("ln1_b", [D])
    w_router = din("w_router", [D, 256]); router_bias = din("router_bias", [256])
    w_exp_gate = din("w_exp_gate", [NE, D, 256]); w_exp_up = din("w_exp_up", [NE, D, 256])
    w_exp_down = din("w_exp_down", [NE, 256, D])
    w_sh_gate = din("w_sh_gate", [D, 256]); w_sh_up = din("w_sh_up", [D, 256]); w_sh_down = din("w_sh_down", [256, D])
    ln2_g = din("ln2_g", [D]); ln2_b = din("ln2_b", [D])
    out = nc.dram_tensor("out", [S, D], F32, kind="ExternalOutput").ap()

    qk_s = dscr("qk_s", [8, 2, 70, S], BF16)
    v_s = dscr("v_s", [S, 8, 65], BF16)
    gq_s = dscr("gq_s", [256, S], F32)
    gk_s = dscr("gk_s", [256, S], F32)
    la_s = dscr("la_s", [256, S], F32)
    gv_s = dscr("gv_s", [S, 512], BF16)
    gr_s = dscr("gr_s", [S, 512], BF16)
    y_s = dscr("y_s", [S, D], BF16)
    h2_s = dscr("h2_s", [S, D], F32)
    h2b_s = dscr("h2b_s", [S, D], BF16)
    ysh_s = dscr("ysh_s", [S, D], F32)
    slot_s = dscr("slot_s", [NE * CAP, 4], U32)
    Y_s = dscr("Y_s", [NE * CAP, D], BF16)
    dbg_dest = dscr("dbg_dest", [128, NT * 8], U32) if debug else None
    NOSCAT = bool(os.environ.get("KC_NOSCAT"))

    arena_t = P.stack.enter_context(nc.sbuf_tensor("arena", [128, ARENA], U8))
    ar = Arena(arena_t, ARENA)
    ps = [P.stack.enter_context(nc.psum_tensor(f"ps{i}", [128, 512], F32)) for i in range(8)]
    Bps = P.bufs(8)

    def psb(i):
        return ps[i][:, :].bitcast(BF16)

    cst = ar.alloc([128, C_END], F32)
    ident_bf = ar.alloc([128, 128], BF16)
    le_bf = ar.alloc([128, 128], BF16)
    lt_bf = ar.alloc([128, 128], BF16)
    ones_bf = ar.alloc([128, 128], BF16)
    destall = ar.alloc([128, NT, 8], U32)
    bscr = ar.alloc([128, 16], F32)
    Bcst = P.buf(); Bdest = P.bufs(NT)
    P.dma("sp", lambda e: e.dma_start(out=cst, in_=cst_d), writes=[Bcst])
    P.dve(lambda e: e.tensor_copy(out=ident_bf, in_=cst[:, C_ID:C_ID + 128]), reads=[Bcst], writes=[Bcst])
    P.dve(lambda e: e.tensor_copy(out=le_bf, in_=cst[:, C_LE:C_LE + 128]), reads=[Bcst], writes=[Bcst])
    P.dve(lambda e: e.tensor_copy(out=lt_bf, in_=cst[:, C_LT:C_LT + 128]), reads=[Bcst], writes=[Bcst])
    P.dve(lambda e: e.memset(ones_bf, 1.0), writes=[Bcst])
    ident_f = cst[:, C_ID:C_ID + 128]
    iota_f = cst[:, C_IOTA:C_IOTA + 256]
    tok_f = cst[:, C_TOK:C_TOK + 32]
    gmark = ar.mark()
    outs = []

    def ln_stats(xt, mv, stt, rstd, Bx, Bmv):
        for hf in range(2):
            P.dve(lambda e, hf=hf: e.bn_stats(out=stt[:, hf, :], in_=xt[:, hf * 512:(hf + 1) * 512]), reads=[Bx], writes=[Bmv])
        P.dve(lambda e: e.bn_aggr(out=mv[:, 0:2], in_=stt.rearrange("p a b -> p (a b)")), reads=[Bmv], writes=[Bmv])
        P.act(lambda e: e.activation(out=rstd, in_=mv[:, 1:2], func=AF.Ln, bias=epsb[:, 0:1], scale=1.0), reads=[Bmv], writes=[Bmv])
        P.act(lambda e: e.activation(out=rstd, in_=rstd, func=AF.Exp, scale=-0.5), reads=[Bmv], writes=[Bmv])

    epsb = ar.alloc([128, 1], F32)
    oneb = ar.alloc([128, 1], F32)
    P.dve(lambda e: e.memset(epsb, EPS), writes=[Bcst])
    P.dve(lambda e: e.memset(oneb, 1.0), writes=[Bcst])
    gmark = ar.mark()

    def phase_A():
        logf = ar.alloc([8, S], F32)
        m2 = ar.mark()
        win = ar.alloc([128, 8, 3096], BF16)
        lnfm = ar.alloc([128, 16], F32)
        hT = [ar.alloc([128, 8, 512], BF16) for _ in range(2)]
        xts = [ar.alloc([128, 1024], F32) for _ in range(3)]
        xhs = [ar.alloc([128, 1024], BF16) for _ in range(2)]
        stts = [ar.alloc([128, 2, 6], F32) for _ in range(3)]
        mvs = [ar.alloc([128, 4], F32) for _ in range(3)]
        qst = [ar.alloc([64, 8, 512], BF16) for _ in range(2)]
        kst = [ar.alloc([64, 8, 512], BF16) for _ in range(2)]
        gst = [ar.alloc([128, 512], F32) for _ in range(4)]
        vst = [ar.alloc([128, 8, 65], BF16) for _ in range(2)]
        gvst = [ar.alloc([128, 512], BF16) for _ in range(2)]
        grst = [ar.alloc([128, 512], BF16) for _ in range(2)]
        gaT = [ar.alloc([17, 512], F32) for _ in range(2)]
        wgu = ar.alloc([17, 256], F32)
        nbf = ar.alloc([8, 1], F32)
        t8 = [ar.alloc([8, 512], F32) for _ in range(2)]
        Bwin = P.buf(); Bln = P.buf(); BhT = [P.bufs(4) for _ in range(2)]
        Bxt = P.bufs(3); Bxh = P.bufs(2); Bmv = P.bufs(3)
        Bq = P.bufs(2); Bk = P.bufs(2); Bg = P.bufs(4); Bv = P.bufs(2); Bgv = P.bufs(2); Bgr = P.bufs(2)
        Blogf = P.buf(); Bga = P.bufs(2); Bwgu = P.buf(); Bt8 = P.bufs(2)

        for c in range(8):
            for j in range(3):
                P.dma("pool", lambda e, c=c, j=j: e.dma_start(out=win[:, c, j * 1032:(j + 1) * 1032],
                                                             in_=w_in[c * 128:(c + 1) * 128, j * 1032:(j + 1) * 1032]), writes=[Bwin])
        P.dma("sp", lambda e: e.dma_start(out=lnfm, in_=lnfm_d), writes=[Bln])
        P.dma("sp", lambda e: e.dma_start(out=wgu[0:16, :], in_=w_gate_up), writes=[Bwgu])
        P.dma("sp", lambda e: e.dma_start(out=wgu[16:17, :], in_=b_gate.rearrange("(o n) -> o n", o=1)), writes=[Bwgu])
        P.dma("sp", lambda e: e.dma_start(out=nbf, in_=b_fgate.rearrange("(p o) -> p o", o=1)), writes=[Bln])
        P.dve(lambda e: e.tensor_scalar(out=nbf, in0=nbf, scalar1=-1.0, scalar2=None, op0=ALU.mult), reads=[Bln], writes=[Bln])
        for i in range(2):
            P.dve(lambda e, i=i: e.memset(gaT[i], 1.0), writes=[Bga[i]])
            P.dve(lambda e, i=i: e.memset(vst[i], 1.0), writes=[Bv[i]])

        bank = [2]

        def nextbank():
            b = bank[0]
            bank[0] = 2 + (bank[0] - 2 + 1) % 6
            return b

        gi = [0]
        def ln_chunk(ch):
            hb = ch % 2
            for t in range(4):
                g = ch * 4 + t
                xt = xts[g % 3]; xh = xhs[g % 2]; mv = mvs[g % 3]; stt = stts[g % 3]
                P.dma("sp" if g < 4 else "pool", lambda e, g=g, xt=xt: e.dma_start(out=xt, in_=x[g * 128:(g + 1) * 128, :]), writes=[Bxt[g % 3]])
                ln_stats(xt, mv, stt, mv[:, 2:3], Bxt[g % 3], Bmv[g % 3])
                P.dve(lambda e, xt=xt, xh=xh, mv=mv: e.tensor_scalar(out=xh, in0=xt, scalar1=mv[:, 0:1], scalar2=mv[:, 2:3],
                                                                     op0=ALU.subtract, op1=ALU.mult),
                      reads=[Bxt[g % 3], Bmv[g % 3]], writes=[Bxh[g % 2]])
                pb = g % 2
                for c in range(8):
                    P.pe(lambda e, c=c, xh=xh, pb=pb: e.transpose(out=psb(pb)[:, c * 128:(c + 1) * 128], in_=xh[:, c * 128:(c + 1) * 128],
                                                                    identity=ident_bf), reads=[Bxh[g % 2], Bcst], writes=[Bps[pb]])
                for c in range(8):
                    P.act(lambda e, c=c, pb=pb, hb=hb, t=t: e.activation(out=hT[hb][:, c, t * 128:(t + 1) * 128],
                                                                          in_=psb(pb)[:, c * 128:(c + 1) * 128], func=AF.Identity,
                                                                          scale=lnfm[:, c:c + 1], bias=lnfm[:, 8 + c:9 + c]),
                          reads=[Bps[pb], Bln], writes=[BhT[hb][t]])

        def proj_chunk(ch):
            hb = ch % 2
            cols = slice(ch * 512, (ch + 1) * 512)

            def fm_group(col0, ncols):
                b = nextbank()
                for c in range(8):
                    P.pe(lambda e, c=c, b=b, hb=hb, col0=col0, ncols=ncols: e.matmul(ps[b][0:ncols, :], lhsT=win[:, c, col0:col0 + ncols], rhs=hT[hb][:, c, :],
                                                      start=(c == 0), stop=(c == 7)),
                         reads=[Bwin] + BhT[hb], writes=[Bps[b]])
                return b

            for which, base, stg, Bst_, sc in (("q", 0, qst, Bq, 0.125), ("k", 512, kst, Bk, 1.0)):
                for i in range(4):
                    b = fm_group(base + i * 128, 128)
                    P.dve(lambda e, b=b, i=i, stg=stg, sc=sc, hb=hb: e.tensor_scalar(out=stg[hb][:, 2 * i, :], in0=ps[b][0:64, :], scalar1=sc,
                                                                               scalar2=None, op0=ALU.mult),
                          reads=[Bps[b]], writes=[Bst_[hb]])
                    P.act(lambda e, b=b, i=i, stg=stg, sc=sc, hb=hb: e.activation(out=stg[hb][:, 2 * i + 1, :], in_=ps[b][64:128, :], func=AF.Copy,
                                                                            scale=sc),
                          reads=[Bps[b]], writes=[Bst_[hb]])
                side = 0 if which == "q" else 1
                P.dma("sp", lambda e, stg=stg, side=side, hb=hb, cols=cols: e.dma_start(out=qk_s[:, side, 0:64, cols].rearrange("h r t -> r h t"), in_=stg[hb]),
                      reads=[Bst_[hb]])
            for base, dst in ((1544, gq_s), (1800, gk_s)):
                for j in range(2):
                    b = fm_group(base + j * 128, 128)
                    k = gi[0] % 4
                    gi[0] += 1
                    P.dve(lambda e, b=b, k=k: e.tensor_copy(out=gst[k], in_=ps[b][:, :]), reads=[Bps[b]], writes=[Bg[k]])
                    P.dma("sp", lambda e, k=k, j=j, dst=dst, cols=cols: e.dma_start(out=dst[j * 128:(j + 1) * 128, cols], in_=gst[k]), reads=[Bg[k]])
            b = fm_group(1536, 8)
            tb = ch % 2
            P.act(lambda e, b=b, tb=tb: e.activation(out=t8[tb], in_=ps[b][0:8, :], func=AF.Exp, scale=-1.0, bias=nbf[:, 0:1]),
                  reads=[Bps[b], Bln], writes=[Bt8[tb]])
            P.act(lambda e, tb=tb: e.activation(out=t8[tb], in_=t8[tb], func=AF.Ln, bias=oneb[0:8, 0:1], scale=1.0), reads=[Bt8[tb]], writes=[Bt8[tb]])
            P.dve(lambda e, tb=tb, cols=cols: e.tensor_scalar(out=logf[:, cols], in0=t8[tb], scalar1=-1.0, scalar2=None, op0=ALU.mult),
                  reads=[Bt8[tb]], writes=[Blogf])
            b = fm_group(2568, 16)
            P.dve(lambda e, b=b, hb=hb: e.tensor_copy(out=gaT[hb][0:16, :], in_=ps[b][0:16, :]), reads=[Bps[b]], writes=[Bga[hb]])
            for j in range(2):
                b = nextbank()
                P.pe(lambda e, b=b, j=j, hb=hb: e.matmul(ps[b][:, :], lhsT=wgu[:, j * 128:(j + 1) * 128], rhs=gaT[hb][:, :], start=True, stop=True),
                     reads=[Bwgu, Bga[hb]], writes=[Bps[b]])
                k = gi[0] % 4
                gi[0] += 1
                P.act(lambda e, b=b, k=k: e.activation(out=gst[k], in_=ps[b][:, :], func=AF.Exp, scale=-1.0), reads=[Bps[b]], writes=[Bg[k]])
                P.act(lambda e, k=k: e.activation(out=gst[k], in_=gst[k], func=AF.Ln, bias=oneb[:, 0:1], scale=1.0), reads=[Bg[k]], writes=[Bg[k]])
                P.dve(lambda e, k=k: e.tensor_scalar(out=gst[k], in0=gst[k], scalar1=-1.0 / 16.0, scalar2=None, op0=ALU.mult),
                      reads=[Bg[k]], writes=[Bg[k]])
                P.dma("sp", lambda e, k=k, j=j, cols=cols: e.dma_start(out=la_s[j * 128:(j + 1) * 128, cols], in_=gst[k]), reads=[Bg[k]])
            for t in range(4):
                g = ch * 4 + t
                rows = slice(g * 128, (g + 1) * 128)
                for col0, kind in ((1024, "fv"), (2056, "gv"), (2584, "gr")):
                    b = nextbank()
                    for c in range(8):
                        P.pe(lambda e, c=c, b=b, t=t, col0=col0, hb=hb: e.matmul(ps[b][:, :], lhsT=hT[hb][:, c, t * 128:(t + 1) * 128],
                                                                           rhs=win[:, c, col0:col0 + 512], start=(c == 0), stop=(c == 7)),
                             reads=[Bwin, BhT[hb][t]], writes=[Bps[b]])
                    k = g % 2
                    if kind == "fv":
                        P.act(lambda e, b=b, k=k: e.activation(out=vst[k][:, :, 0:64], in_=ps[b][:, :].rearrange("p (h d) -> p h d", h=8),
                                                               func=AF.Copy), reads=[Bps[b]], writes=[Bv[k]])
                        P.dma("sp", lambda e, k=k, rows=rows: e.dma_start(out=v_s[rows, :, :], in_=vst[k]), reads=[Bv[k]])
                    elif kind == "gv":
                        P.dve(lambda e, b=b, k=k: e.tensor_copy(out=gvst[k], in_=ps[b][:, :]), reads=[Bps[b]], writes=[Bgv[k]])
                        P.dma("sp", lambda e, k=k, rows=rows: e.dma_start(out=gv_s[rows, :], in_=gvst[k]), reads=[Bgv[k]])
                    else:
                        P.act(lambda e, b=b, k=k: e.activation(out=grst[k], in_=ps[b][:, :], func=AF.Copy), reads=[Bps[b]], writes=[Bgr[k]])
                        P.dma("sp", lambda e, k=k, rows=rows: e.dma_start(out=gr_s[rows, :], in_=grst[k]), reads=[Bgr[k]])
        ln_chunk(0)
        for ch in range(8):
            P.cap_begin()
            if ch + 1 < 8:
                ln_chunk(ch + 1)
            la = P.cap_end()
            P.cap_begin()
            proj_chunk(ch)
            lb = P.cap_end()
            P.interleave(la, lb)
        P.barrier(bscr)
        ar.release(m2)
        ones8 = ar.alloc([8, S], BF16)
        cT = ar.alloc([8, S], F32)
        r1 = ar.alloc([8, S], F32)
        parts = [ar.alloc([8, S], BF16) for _ in range(3)]
        negs = [ar.alloc([8, S], BF16) for _ in range(3)]
        Bc = P.buf(); Bpart = P.bufs(3); Bneg = P.bufs(3); Bone = P.buf()
        P.dve(lambda e: e.memset(ones8, 1.0), writes=[Bone])
        P.dve(lambda e: e.tensor_tensor_scan(out=cT, data0=ones8, data1=logf, initial=0.0, op0=ALU.mult, op1=ALU.add),
              reads=[Blogf, Bone], writes=[Bc])
        src = cT
        for i in range(3):
            P.dve(lambda e, i=i, src=src: e.tensor_copy(out=parts[i], in_=src), reads=[Bc], writes=[Bpart[i]])
            if i < 2:
                dst = r1
                P.dve(lambda e, i=i, src=src, dst=dst: e.tensor_tensor(out=dst, in0=src, in1=parts[i], op=ALU.subtract),
                      reads=[Bc, Bpart[i]], writes=[Bc])
                src = r1
            P.dve(lambda e, i=i: e.tensor_scalar(out=negs[i], in0=parts[i], scalar1=-1.0, scalar2=None, op0=ALU.mult),
                  reads=[Bpart[i]], writes=[Bneg[i]])
        for r in range(6):
            qsrc, qb = (parts[r], Bpart[r]) if r < 3 else (ones8, Bone)
            ksrc, kb = (ones8, Bone) if r < 3 else (negs[r - 3], Bneg[r - 3])
            P.dma("sp", lambda e, r=r, qsrc=qsrc: e.dma_start(out=qk_s[:, 0, 64 + r, :], in_=qsrc), reads=[qb])
            P.dma("sp", lambda e, r=r, ksrc=ksrc: e.dma_start(out=qk_s[:, 1, 64 + r, :], in_=ksrc), reads=[kb])
        P.barrier(bscr)
        ar.release(gmark)

    def phase_B():
        qT = [ar.alloc([70, S], BF16) for _ in range(2)]
        kT = [ar.alloc([70, S], BF16) for _ in range(2)]
        vh = [ar.alloc([128, NT, 65], BF16) for _ in range(2)]
        pT = [ar.alloc([128, 512], BF16) for _ in range(3)]
        yh = [ar.alloc([128, NT, 64], BF16) for _ in range(2)]
        rec = [ar.alloc([128, 4], F32) for _ in range(2)]
        Bq = P.bufs(2); Bk = P.bufs(2); Bv = P.bufs(2); BpT = P.bufs(3); Byh = P.bufs(2); Brec = P.bufs(2)
        v_view = v_s.rearrange("(n p) h e -> p n h e", p=128)
        y_view = y_s.rearrange("(n p) c -> p n c", p=128)
        SB = [0, 1, 4]
        units = [(h, c, j) for h in range(8) for c in range(8) for j in range(4 * c + 4)]

        def geom(u):
            h, c, j = u
            i_lo = max(4 * c, j)
            nblk = 4 * c + 4 - i_lo
            return i_lo, nblk, nblk * 128

        def emit_load(h):
            hb = h % 2
            P.dma("sp", lambda e, h=h, hb=hb: e.dma_start(out=qT[hb], in_=qk_s[h, 0, :, :]), writes=[Bq[hb]])
            P.dma("sp", lambda e, h=h, hb=hb: e.dma_start(out=kT[hb], in_=qk_s[h, 1, :, :]), writes=[Bk[hb]])
            for q4 in range(4):
                P.dma("sp", lambda e, h=h, hb=hb, q4=q4: e.dma_start(out=vh[hb][:, q4 * 8:(q4 + 1) * 8, :],
                                                                    in_=v_view[:, q4 * 8:(q4 + 1) * 8, h, :]), writes=[Bv[hb]])

        def emit_mm(u, k):
            h, c, j = u
            hb = h % 2
            i_lo, nblk, ncols = geom(u)
            sb = SB[k % 3]
            P.pe(lambda e, hb=hb, j=j, i_lo=i_lo, ncols=ncols, sb=sb: e.matmul(
                ps[sb][:, 0:ncols], lhsT=kT[hb][:, j * 128:(j + 1) * 128], rhs=qT[hb][:, i_lo * 128:i_lo * 128 + ncols],
                start=True, stop=True), reads=[Bq[hb], Bk[hb]], writes=[Bps[sb]])

        def emit_rest(u, k):
            h, c, j = u
            hb = h % 2
            i_lo, nblk, ncols = geom(u)
            sb = SB[k % 3]
            pb = k % 3
            iu = h * 8 + c
            ab = 2 + iu % 2
            rb = iu % 2
            acc = ps[ab][:, 0:260].rearrange("p (b e) -> p b e", b=4)
            P.act(lambda e, sb=sb, pb=pb, ncols=ncols: e.activation(out=pT[pb][:, 0:ncols], in_=ps[sb][:, 0:ncols], func=AF.Exp),
                  reads=[Bps[sb]], writes=[BpT[pb]])
            if j >= 4 * c:
                P.dve(lambda e, pb=pb: e.tensor_tensor(out=pT[pb][:, 0:128], in0=pT[pb][:, 0:128], in1=le_bf, op=ALU.mult),
                      reads=[BpT[pb], Bcst], writes=[BpT[pb]])
            for bi in range(nblk):
                i = i_lo + bi
                blk = i - 4 * c
                P.pe(lambda e, pb=pb, bi=bi, blk=blk, hb=hb, j=j, acc=acc, first=(j == 0 and blk == 0), last=(j == i): e.matmul(
                    acc[:, blk, :], lhsT=pT[pb][:, bi * 128:(bi + 1) * 128], rhs=vh[hb][:, j, :],
                    start=first, stop=last, skip_group_check=True), reads=[BpT[pb], Bv[hb]], writes=[Bps[ab]])
            if j == 4 * c + 3:
                P.dve(lambda e, acc=acc, rb=rb: e.reciprocal(out=rec[rb], in_=acc[:, :, 64]), reads=[Bps[ab]], writes=[Brec[rb]])
                for blk in range(4):
                    P.dve(lambda e, acc=acc, rb=rb, blk=blk, hb=hb, c=c: e.tensor_scalar(
                        out=yh[hb][:, 4 * c + blk, :], in0=acc[:, blk, 0:64], scalar1=rec[rb][:, blk:blk + 1], scalar2=None, op0=ALU.mult),
                        reads=[Bps[ab], Brec[rb]], writes=[Byh[hb]])
                if c == 7:
                    for q4 in range(4):
                        P.dma("sp", lambda e, h=h, hb=hb, q4=q4: e.dma_start(out=y_view[:, q4 * 8:(q4 + 1) * 8, h * 64:(h + 1) * 64],
                                                                            in_=yh[hb][:, q4 * 8:(q4 + 1) * 8, :]), reads=[Byh[hb]])

        emit_load(0)
        emit_load(1)
        emit_mm(units[0], 0)
        emit_mm(units[1], 1)
        for k, u in enumerate(units):
            if k + 2 < len(units):
                emit_mm(units[k + 2], k + 2)
            emit_rest(u, k)
            if u[1] == 7 and u[2] == 31 and u[0] + 2 < 8:
                emit_load(u[0] + 2)
        P.barrier(bscr)
        ar.release(gmark)

    def phase_C():
        BL = 512
        resetm8 = ar.alloc([64, 8, 64], BF16)
        mask8 = ar.alloc([64, 8, 64], BF16)
        gnb = ar.alloc([64, 128], F32)
        Bm = P.buf()
        P.dve(lambda e: e.memset(resetm8, 1.0), writes=[Bm])
        P.dve(lambda e: e.memset(resetm8[:, :, 0:1], 0.0), writes=[Bm])
        for q in range(8):
            P.dve(lambda e, q=q: e.tensor_copy(out=mask8[:, q, :], in_=le_bf[0:64, 0:64]), reads=[Bcst], writes=[Bm])
        P.dma("sp", lambda e: e.dma_start(out=gnb, in_=g_gla_norm.partition_broadcast(64)), writes=[Bm])
        gv_view = gv_s.rearrange("(n s) e -> s n e", s=64)
        gr_view = gr_s.rearrange("(n s) e -> s n e", s=64)
        y_view = y_s.rearrange("(n s) c -> s n c", s=64)
        H = []
        for hg in range(4):
            d = dict(S1=ar.alloc([64, BL], F32), S2=ar.alloc([64, BL], F32), S3=ar.alloc([64, BL], F32), S4=ar.alloc([64, BL], F32),
                     qt=ar.alloc([64, BL], BF16), kt=ar.alloc([64, BL], BF16), khT=ar.alloc([64, 8, 64], BF16),
                     khat=ar.alloc([64, 8, 64], BF16), gvp=ar.alloc([64, 8, 128], BF16), kvall=ar.alloc([64, 8, 128], F32),
                     ebl=ar.alloc([64, 8], F32), ost=[ar.alloc([64, 8, 128], F32) for _ in range(2)], grp=[ar.alloc([64, 8, 128], BF16) for _ in range(2)],
                     sq=ar.alloc([64, 1024], F32), sg=ar.alloc([64, 1024], F32), ss=ar.alloc([64, 8], F32),
                     yg=ar.alloc([64, 8, 128], BF16), state=ar.alloc([64, 128], F32),
                     sbf=[ar.alloc([64, 128], BF16) for _ in range(2)])
            d["B"] = {k: P.buf() for k in ("S1", "S2", "S3", "S4", "qt", "kt", "khT", "khat", "gvp", "kvall", "ebl", "ost0", "ost1", "grp0", "grp1", "sq", "sg",
                                             "ss", "yg", "state", "sbf0", "sbf1")}
            H.append(d)
            P.dve(lambda e, d=d: e.memset(d["state"], 0.0), writes=[d["B"]["state"]])
            P.dve(lambda e, d=d: e.memset(d["sbf"][0], 0.0), writes=[d["B"]["sbf0"]])

        def head_block(hg, bk, part):
            d = H[hg]; B = d["B"]
            par = bk % 2
            Bost = B["ost%d" % par]; Bgrp = B["grp%d" % par]
            S1, S2, S3, S4, qt, kt, khT, khat, gvp, kvall = (d[k] for k in ("S1", "S2", "S3", "S4", "qt", "kt", "khT", "khat", "gvp", "kvall"))
            ebl, sq, sg, ss, yg, state, sbf = (d[k] for k in ("ebl", "sq", "sg", "ss", "yg", "state", "sbf"))
            ost = d["ost"][par]; grp = d["grp"][par]
            Bsbf = [B["sbf0"], B["sbf1"]]
            rows = slice(hg * 64, (hg + 1) * 64)
            es = slice(hg * 128, (hg + 1) * 128)
            cols = slice(bk * BL, (bk + 1) * BL)
            ns = slice(bk * 8, (bk + 1) * 8)
            pa, pb = 2 * hg, 2 * hg + 1
            def post():
                o = ost.rearrange("p a b -> p (a b)")
                grf = grp.rearrange("p a b -> p (a b)")
                for a8 in range(8):
                    P.act(lambda e, a8=a8: e.activation(out=sq[:, a8 * 128:(a8 + 1) * 128], in_=o[:, a8 * 128:(a8 + 1) * 128], func=AF.Square,
                                                        accum_out=ss[:, a8:a8 + 1]), reads=[Bost], writes=[B["sq"], B["ss"]])
                P.act(lambda e: e.activation(out=ss, in_=ss, func=AF.Ln, scale=1.0 / 128.0, bias=epsb[0:64, 0:1]), reads=[B["ss"]], writes=[B["ss"]])
                P.act(lambda e: e.activation(out=ss, in_=ss, func=AF.Exp, scale=-0.5), reads=[B["ss"]], writes=[B["ss"]])
                P.act(lambda e: e.activation(out=sg, in_=grf, func=AF.Exp, scale=-1.0), reads=[Bgrp], writes=[B["sg"]])
                P.dve(lambda e: e.tensor_scalar(out=sg, in0=sg, scalar1=1.0, scalar2=None, op0=ALU.add), reads=[B["sg"]], writes=[B["sg"]])
                P.dve(lambda e: e.reciprocal(out=sg, in_=sg), reads=[B["sg"]], writes=[B["sg"]])
                P.dve(lambda e: e.tensor_tensor(out=sg, in0=sg, in1=grf, op=ALU.mult), reads=[B["sg"], Bgrp], writes=[B["sg"]])
                P.dve(lambda e: e.tensor_tensor(out=sg.rearrange("p (a b) -> p a b", b=128), in0=sg.rearrange("p (a b) -> p a b", b=128),
                                                in1=gnb.unsqueeze(1).to_broadcast([64, 8, 128]), op=ALU.mult), reads=[B["sg"], Bm], writes=[B["sg"]])
                P.dve(lambda e: e.tensor_tensor(out=sq.rearrange("p (a b) -> p a b", b=128), in0=o.rearrange("p (a b) -> p a b", b=128),
                                                in1=ss.unsqueeze(2).to_broadcast([64, 8, 128]), op=ALU.mult), reads=[Bost, B["ss"]], writes=[B["sq"]])
                P.dve(lambda e: e.tensor_tensor(out=yg.rearrange("p a b -> p (a b)"), in0=sq, in1=sg, op=ALU.mult), reads=[B["sq"], B["sg"]], writes=[B["yg"]])
                P.dma("sp", lambda e: e.dma_start(out=y_view[:, ns, 512 + hg * 128:512 + (hg + 1) * 128], in_=yg), reads=[B["yg"]])

            if part == "post":
                return post()
            P.dma("pool", lambda e: e.dma_start(out=S1, in_=gq_s[rows, cols]), writes=[B["S1"]])
            P.dma("pool", lambda e: e.dma_start(out=S2, in_=gk_s[rows, cols]), writes=[B["S2"]])
            P.dma("pool", lambda e: e.dma_start(out=S3, in_=la_s[rows, cols]), writes=[B["S3"]])
            P.dma("pool", lambda e: e.dma_start(out=gvp, in_=gv_view[:, ns, es]), writes=[B["gvp"]])
            P.dma("pool", lambda e: e.dma_start(out=grp, in_=gr_view[:, ns, es]), writes=[Bgrp])
            S4v = S4.rearrange("p (n t) -> p n t", t=64)
            ktv = kt.rearrange("p (n t) -> p n t", t=64)
            P.dve(lambda e: e.tensor_tensor_scan(out=S4, data0=resetm8.rearrange("p a b -> p (a b)"), data1=S3, initial=0.0,
                                                 op0=ALU.mult, op1=ALU.add), reads=[B["S3"], Bm], writes=[B["S4"]])
            P.act(lambda e: e.activation(out=ebl, in_=S4v[:, :, 63], func=AF.Exp), reads=[B["S4"]], writes=[B["ebl"]])
            P.act(lambda e: e.activation(out=S3, in_=S4, func=AF.Exp), reads=[B["S4"]], writes=[B["S3"]])
            P.dve(lambda e: e.scalar_tensor_tensor(out=qt, in0=S1, scalar=0.125, in1=S3, op0=ALU.mult, op1=ALU.mult),
                  reads=[B["S1"], B["S3"]], writes=[B["qt"]])
            P.act(lambda e: e.activation(out=S3, in_=S4, func=AF.Exp, scale=-1.0), reads=[B["S4"]], writes=[B["S3"]])
            P.dve(lambda e: e.tensor_tensor(out=kt, in0=S2, in1=S3, op=ALU.mult), reads=[B["S2"], B["S3"]], writes=[B["kt"]])
            P.dve(lambda e: e.tensor_tensor(out=khT, in0=ktv, in1=ebl.unsqueeze(2).to_broadcast([64, 8, 64]), op=ALU.mult),
                  reads=[B["kt"], B["ebl"]], writes=[B["khT"]])
            for n in range(8):
                P.pe(lambda e, n=n: e.transpose(out=psb(pa)[0:64, n * 64:(n + 1) * 64], in_=khT[:, n, :], identity=ident_bf[0:64, 0:64]),
                     reads=[B["khT"], Bcst], writes=[Bps[pa]])
            P.act(lambda e: e.activation(out=khat.rearrange("p a b -> p (a b)"), in_=psb(pa)[0:64, 0:512], func=AF.Copy),
                  reads=[Bps[pa]], writes=[B["khat"]])
            for n in range(8):
                cs = slice(n * 64, (n + 1) * 64)
                P.pe(lambda e, n=n, cs=cs: e.matmul(ps[pa][0:64, n * 64:(n + 1) * 64], lhsT=kt[:, cs], rhs=qt[:, cs], start=True, stop=True,
                                                    skip_group_check=True), reads=[B["qt"], B["kt"]], writes=[Bps[pa]])
            P.dve(lambda e: e.tensor_tensor(out=khT, in0=ps[pa][0:64, 0:512].rearrange("p (a b) -> p a b", b=64), in1=mask8, op=ALU.mult),
                  reads=[Bps[pa], Bm], writes=[B["khT"]])
            for i in range(2):
                for q in range(4):
                    n = 4 * i + q
                    P.pe(lambda e, n=n, q=q: e.matmul(ps[pb][0:64, q * 128:(q + 1) * 128], lhsT=khat[:, n, :], rhs=gvp[:, n, :], start=True, stop=True,
                                                      skip_group_check=True), reads=[B["khat"], B["gvp"]], writes=[Bps[pb]])
                P.act(lambda e, i=i: e.activation(out=kvall[:, 4 * i:4 * i + 4, :].rearrange("p a b -> p (a b)"), in_=ps[pb][0:64, :], func=AF.Copy),
                      reads=[Bps[pb]], writes=[B["kvall"]])
            for n in range(8):
                cs = slice(n * 64, (n + 1) * 64)
                ob = pa if n % 2 == 0 else pb
                P.pe(lambda e, n=n, ob=ob: e.matmul(ps[ob][0:64, 0:128], lhsT=khT[:, n, :], rhs=gvp[:, n, :], start=True, stop=False),
                     reads=[B["khT"], B["gvp"]], writes=[Bps[ob]])
                P.pe(lambda e, n=n, cs=cs, ob=ob: e.matmul(ps[ob][0:64, 0:128], lhsT=qt[:, cs], rhs=sbf[n % 2], start=False, stop=True),
                     reads=[B["qt"], Bsbf[n % 2]], writes=[Bps[ob]])
                P.act(lambda e, n=n, ob=ob: e.activation(out=ost[:, n, :], in_=ps[ob][0:64, 0:128], func=AF.Copy), reads=[Bps[ob]], writes=[Bost])
                P.dve(lambda e, n=n: e.scalar_tensor_tensor(out=state, in0=state, scalar=ebl[:, n:n + 1], in1=kvall[:, n, :], op0=ALU.mult, op1=ALU.add),
                      reads=[B["kvall"], B["ebl"], B["state"]], writes=[B["state"]])
                P.act(lambda e, n=n: e.activation(out=sbf[(n + 1) % 2], in_=state, func=AF.Copy), reads=[B["state"]], writes=[Bsbf[(n + 1) % 2]])
        def cap_hb(hg, bk, part):
            P.cap_begin(); head_block(hg, bk, part); return P.cap_end()

        nblk = S // BL
        for bk in range(nblk + 1):
            lists = []
            if bk < nblk:
                lists += [cap_hb(hg, bk, "main") for hg in range(4)]
            if bk > 0:
                lists += [cap_hb(hg, bk - 1, "post") for hg in range(4)]
            P.interleave(*lists)
        P.barrier(bscr)
        ar.release(gmark)

    def bcast_load(dst, vec, B):
        P.dma("sp", lambda e: e.dma_start(out=dst, in_=vec.partition_broadcast(128)), writes=[B])

    def phase_D():
        wout = ar.alloc([128, 8, 1024], BF16)
        g0b = ar.alloc([128, 1024], F32); b0b = ar.alloc([128, 1024], F32)
        g1b = ar.alloc([128, 1024], F32); b1b = ar.alloc([128, 1024], F32)
        wr = ar.alloc([128, 8, 256], F32)
        rbias = ar.alloc([128, 256], F32)
        wshgu = ar.alloc([128, 8, 512], BF16)
        wshd = ar.alloc([128, 2, 1024], BF16)
        posbase = ar.alloc([128, 256], F32)
        zt = ar.alloc([128, 1024], U32)
        yt = [ar.alloc([128, 1024], BF16) for _ in range(2)]
        yT = [ar.alloc([128, 8, 128], BF16) for _ in range(2)]
        xt = [ar.alloc([128, 1024], F32) for _ in range(2)]
        hh = [ar.alloc([128, 1024], F32) for _ in range(2)]
        h2 = [ar.alloc([128, 1024], F32) for _ in range(2)]
        h2T = [ar.alloc([128, 8, 128], BF16) for _ in range(2)]
        h2Tb = [ar.alloc([128, 8, 128], BF16) for _ in range(2)]
        h2hi = [ar.alloc([128, 1024], BF16) for _ in range(2)]
        h2lo = [ar.alloc([128, 1024], BF16) for _ in range(2)]
        wrh = ar.alloc([128, 8, 256], BF16); wrl = ar.alloc([128, 8, 256], BF16)
        ysh = [ar.alloc([128, 1024], F32) for _ in range(2)]
        stt = [ar.alloc([128, 2, 6], F32) for _ in range(2)]
        stt2 = [ar.alloc([128, 2, 6], F32) for _ in range(2)]
        mv2 = [ar.alloc([128, 8], F32) for _ in range(2)]
        Bmv2 = P.bufs(2)
        mv = [ar.alloc([128, 8], F32) for _ in range(2)]
        sc0 = ar.alloc([128, 256], F32); scores = ar.alloc([128, 256], F32); biased = ar.alloc([128, 256], F32)
        mb = ar.alloc([128, 256], F32); sel = ar.alloc([128, 256], BF16); wsel = ar.alloc([128, 256], F32)
        G = ar.alloc([128, 256], F32); pos = ar.alloc([128, 256], F32); junk = ar.alloc([128, 256], F32)
        top8g = ar.alloc([128, 8, 8], F32); grp = ar.alloc([128, 8], F32); g8 = ar.alloc([128, 8], F32)
        gmask = ar.alloc([128, 8], F32); v8 = ar.alloc([128, 8], F32); i8 = ar.alloc([128, 8], U32)
        i8f = ar.alloc([128, 8], F32); psel = ar.alloc([128, 8], F32); gsel = ar.alloc([128, 8], F32)
        destf = ar.alloc([128, 8], F32); tmpc = ar.alloc([128, 8], F32); sumw = ar.alloc([128, 2], F32)
        info = [ar.alloc([128, 8, 4], U32) for _ in range(2)]
        dslot = [ar.alloc([128, 8], U32) for _ in range(2)]
        tfl = ar.alloc([128, 8], F32); ppos = ar.alloc([128, 8], F32)
        Bds = P.bufs(2)
        Binfo0 = P.bufs(2)
        sA = ar.alloc([128, 256], F32); aT = ar.alloc([128, 256], BF16)
        Bw = P.buf(); Bpb = P.buf(); Bz = P.buf()
        Byt = P.bufs(2); ByT = P.bufs(2); Bxt = P.bufs(2); Bhh = P.bufs(2); Bh2 = P.bufs(2); Bh2T = P.bufs(2); Bh2Tb = P.bufs(2)
        Bysh = P.bufs(2); Bmv = P.bufs(2); Br = P.buf(); Binfo = P.bufs(2); BsA = P.buf(); BaT = P.buf()
        for c in range(8):
            P.dma("pool", lambda e, c=c: e.dma_start(out=wout[:, c, :], in_=w_out[c * 128:(c + 1) * 128, :]), writes=[Bw])
        bcast_load(g0b, ln_in_g, Bw); bcast_load(b0b, ln_in_b, Bw); bcast_load(g1b, ln1_g, Bw); bcast_load(b1b, ln1_b, Bw)
        bcast_load(rbias, router_bias, Bw)
        P.dma("sp", lambda e: e.dma_start(out=wr, in_=w_router.rearrange("(c p) e -> p c e", p=128)), writes=[Bw])
        P.dma("pool", lambda e: e.dma_start(out=wshgu[:, :, 0:256], in_=w_sh_gate.rearrange("(c p) f -> p c f", p=128)), writes=[Bw])
        P.dma("pool", lambda e: e.dma_start(out=wshgu[:, :, 256:512], in_=w_sh_up.rearrange("(c p) f -> p c f", p=128)), writes=[Bw])
        P.dma("pool", lambda e: e.dma_start(out=wshd, in_=w_sh_down.rearrange("(c p) d -> p c d", p=128)), writes=[Bw])
        P.dve(lambda e: e.memset(posbase, 0.0), writes=[Bpb])
        for i in range(2):
            P.dve(lambda e, i=i: e.memset(info[i].bitcast(F32), 0.0), writes=[Binfo[i]])
        P.dve(lambda e: e.tensor_copy(out=wrh, in_=wr), reads=[Bw], writes=[Bw])
        P.dve(lambda e: e.tensor_tensor(out=wrl, in0=wr, in1=wrh, op=ALU.subtract), reads=[Bw], writes=[Bw])
        Bhl = P.bufs(2)
        ab0row = ar.alloc([1, 1024], F32); onesrow = ar.alloc([1, 128], F32)
        P.dma("sp", lambda e: e.dma_start(out=ab0row, in_=ln_in_b.rearrange("(o n) -> o n", o=1)), writes=[Bw])
        P.dve(lambda e: e.tensor_scalar(out=ab0row, in0=ab0row, scalar1=ALPHA, scalar2=None, op0=ALU.mult), reads=[Bw], writes=[Bw])
        P.dve(lambda e: e.tensor_scalar(out=g0b, in0=g0b, scalar1=ALPHA, scalar2=None, op0=ALU.mult), reads=[Bw], writes=[Bw])
        P.dve(lambda e: e.memset(onesrow, 1.0), writes=[Bw])
        P.dma("sp", lambda e: e.dma_start(out=slot_s.rearrange("(p n) w -> p (n w)", p=128), in_=slot_init_d), writes=[Bz])
        def part1a(g):
            pb = g % 2
            lb = 4 + g % 2
            rows = slice(g * 128, (g + 1) * 128)
            P.stage = 1
            P.dma("sp", lambda e, pb=pb, rows=rows: e.dma_start(out=yt[pb], in_=y_s[rows, :]), writes=[Byt[pb]])
            P.dma("sp", lambda e, pb=pb, rows=rows: e.dma_start(out=xt[pb], in_=x[rows, :]), writes=[Bxt[pb]])
            for c in range(8):
                P.pe(lambda e, c=c, pb=pb: e.transpose(out=psb(0)[:, c * 128:(c + 1) * 128], in_=yt[pb][:, c * 128:(c + 1) * 128], identity=ident_bf),
                     reads=[Byt[pb], Bcst], writes=[Bps[0]])
            P.act(lambda e, pb=pb: e.activation(out=yT[pb].rearrange("p a b -> p (a b)"), in_=psb(0)[:, :], func=AF.Copy),
                  reads=[Bps[0]], writes=[ByT[pb]])
            for hf in range(2):
                P.pe(lambda e, hf=hf: e.matmul(ps[1 + hf][:, :], lhsT=onesrow, rhs=ab0row[:, hf * 512:(hf + 1) * 512], start=True, stop=False,
                                               skip_group_check=True), reads=[Bw], writes=[Bps[1 + hf]])
                for c in range(8):
                    P.pe(lambda e, c=c, pb=pb, hf=hf: e.matmul(ps[1 + hf][:, :], lhsT=yT[pb][:, c, :], rhs=wout[:, c, hf * 512:(hf + 1) * 512],
                                                              start=False, stop=(c == 7), skip_group_check=True),
                         reads=[ByT[pb], Bw], writes=[Bps[1 + hf]])
            P.stage = 2
            ln_stats(xt[pb], mv[pb], stt[pb], mv[pb][:, 2:3], Bxt[pb], Bmv[pb])
            P.dve(lambda e, pb=pb: e.scalar_tensor_tensor(out=mv[pb][:, 3:4], in0=mv[pb][:, 0:1], scalar=-1.0, in1=mv[pb][:, 2:3],
                                                          op0=ALU.mult, op1=ALU.mult), reads=[Bmv[pb]], writes=[Bmv[pb]])
            P.act(lambda e, pb=pb: e.activation(out=hh[pb], in_=xt[pb], func=AF.Identity, scale=mv[pb][:, 2:3], bias=mv[pb][:, 3:4]),
                  reads=[Bxt[pb], Bmv[pb]], writes=[Bhh[pb]])
            P.dve(lambda e, pb=pb: e.tensor_tensor(out=hh[pb], in0=hh[pb], in1=g0b, op=ALU.mult), reads=[Bhh[pb], Bw], writes=[Bhh[pb]])
            for hf in range(2):
                P.dve(lambda e, pb=pb, hf=hf: e.tensor_tensor(out=hh[pb][:, hf * 512:(hf + 1) * 512], in0=hh[pb][:, hf * 512:(hf + 1) * 512],
                                                              in1=ps[1 + hf][:, :], op=ALU.add),
                      reads=[Bhh[pb], Bps[1 + hf]], writes=[Bhh[pb]])

        def part1b(g):
            pb = g % 2
            lb = 4 + g % 2
            rows = slice(g * 128, (g + 1) * 128)
            ln_stats(hh[pb], mv2[pb][:, 4:8], stt2[pb], mv2[pb][:, 6:7], Bhh[pb], Bmv2[pb])
            P.dve(lambda e, pb=pb: e.scalar_tensor_tensor(out=mv2[pb][:, 7:8], in0=mv2[pb][:, 4:5], scalar=-1.0, in1=mv2[pb][:, 6:7],
                                                          op0=ALU.mult, op1=ALU.mult), reads=[Bmv2[pb]], writes=[Bmv2[pb]])
            P.act(lambda e, pb=pb: e.activation(out=h2[pb], in_=hh[pb], func=AF.Identity, scale=mv2[pb][:, 6:7], bias=mv2[pb][:, 7:8]),
                  reads=[Bhh[pb], Bmv2[pb]], writes=[Bh2[pb]])
            P.dve(lambda e, pb=pb: e.tensor_tensor(out=h2[pb], in0=h2[pb], in1=g1b, op=ALU.mult), reads=[Bh2[pb], Bw], writes=[Bh2[pb]])
            P.dve(lambda e, pb=pb: e.tensor_tensor(out=h2[pb], in0=h2[pb], in1=b1b, op=ALU.add), reads=[Bh2[pb], Bw], writes=[Bh2[pb]])
            P.dma("sp", lambda e, pb=pb, rows=rows: e.dma_start(out=h2_s[rows, :], in_=h2[pb]), reads=[Bh2[pb]])
            P.dma("pool", lambda e, pb=pb, rows=rows: e.dma_start(out=h2b_s[rows, :], in_=h2[pb]), reads=[Bh2[pb]])
            P.stage = 3
            P.act(lambda e, pb=pb: e.activation(out=h2hi[pb], in_=h2[pb], func=AF.Copy), reads=[Bh2[pb]], writes=[Bhl[pb]])
            P.dve(lambda e, pb=pb: e.tensor_tensor(out=h2lo[pb], in0=h2[pb], in1=h2hi[pb], op=ALU.subtract), reads=[Bh2[pb], Bhl[pb]], writes=[Bhl[pb]])
            for c in range(8):
                P.pe(lambda e, c=c, pb=pb: e.transpose(out=psb(3)[:, c * 128:(c + 1) * 128], in_=h2hi[pb][:, c * 128:(c + 1) * 128], identity=ident_bf),
                     reads=[Bhl[pb], Bcst], writes=[Bps[3]])
            P.act(lambda e, pb=pb: e.activation(out=h2Tb[pb].rearrange("p a b -> p (a b)"), in_=psb(3)[:, :], func=AF.Copy),
                  reads=[Bps[3]], writes=[Bh2Tb[pb]])
            for c in range(8):
                P.pe(lambda e, c=c, pb=pb: e.transpose(out=psb(3)[:, c * 128:(c + 1) * 128], in_=h2lo[pb][:, c * 128:(c + 1) * 128], identity=ident_bf),
                     reads=[Bhl[pb], Bcst], writes=[Bps[3]])
            P.act(lambda e, pb=pb: e.activation(out=h2T[pb].rearrange("p a b -> p (a b)"), in_=psb(3)[:, :], func=AF.Copy),
                  reads=[Bps[3]], writes=[Bh2T[pb]])
            combos = [(h2Tb, wrh), (h2T, wrh), (h2Tb, wrl)]
            for ci, (lt_, wt_) in enumerate(combos):
                for c in range(8):
                    P.pe(lambda e, c=c, pb=pb, lt_=lt_, wt_=wt_, ci=ci, lb=lb: e.matmul(ps[lb][:, 0:256], lhsT=lt_[pb][:, c, :], rhs=wt_[:, c, :],
                                                                                start=(ci == 0 and c == 0), stop=(ci == 2 and c == 7),
                                                                                skip_group_check=True),
                         reads=[Bh2T[pb], Bh2Tb[pb], Bw], writes=[Bps[lb]])

        def part2a(g):
            pb = g % 2
            lb = 4 + g % 2
            rows = slice(g * 128, (g + 1) * 128)
            P.stage = 5
            for fc in range(4):
                for c in range(8):
                    P.pe(lambda e, c=c, fc=fc, pb=pb: e.matmul(ps[7][:, fc * 128:(fc + 1) * 128], lhsT=wshgu[:, c, fc * 128:(fc + 1) * 128],
                                                              rhs=h2Tb[pb][:, c, :], start=(c == 0), stop=(c == 7), skip_group_check=True),
                         reads=[Bh2Tb[pb], Bw], writes=[Bps[7]])
            P.act(lambda e: e.activation(out=sA, in_=ps[7][:, 0:256], func=AF.Exp, scale=-1.0), reads=[Bps[7]], writes=[BsA])
            P.dve(lambda e: e.tensor_scalar(out=sA, in0=sA, scalar1=1.0, scalar2=None, op0=ALU.add), reads=[BsA], writes=[BsA])
            P.dve(lambda e: e.reciprocal(out=sA, in_=sA), reads=[BsA], writes=[BsA])
            P.dve(lambda e: e.tensor_tensor(out=sA, in0=sA, in1=ps[7][:, 0:256], op=ALU.mult), reads=[BsA, Bps[7]], writes=[BsA])
            P.dve(lambda e: e.tensor_tensor(out=aT, in0=sA, in1=ps[7][:, 256:512], op=ALU.mult), reads=[BsA, Bps[7]], writes=[BaT])
            for hf in range(2):
                for f2 in range(2):
                    P.pe(lambda e, hf=hf, f2=f2: e.matmul(ps[7][:, :], lhsT=aT[:, f2 * 128:(f2 + 1) * 128], rhs=wshd[:, f2, hf * 512:(hf + 1) * 512],
                                                         start=(f2 == 0), stop=(f2 == 1)), reads=[BaT, Bw], writes=[Bps[7]])
                P.act(lambda e, hf=hf, pb=pb: e.activation(out=ysh[pb][:, hf * 512:(hf + 1) * 512], in_=ps[7][:, :], func=AF.Copy),
                      reads=[Bps[7]], writes=[Bysh[pb]])
            P.dma("sp", lambda e, pb=pb, rows=rows: e.dma_start(out=ysh_s[rows, :], in_=ysh[pb]), reads=[Bysh[pb]])

        def part2b(g):
            pb = g % 2
            lb = 4 + g % 2
            rows = slice(g * 128, (g + 1) * 128)
            P.stage = 4
            R = [Br]
            P.act(lambda e, lb=lb: e.activation(out=sc0, in_=ps[lb][:, 0:256], func=AF.Exp, scale=-1.0), reads=[Bps[lb]], writes=R)
            P.dve(lambda e: e.tensor_scalar(out=sc0, in0=sc0, scalar1=1.0, scalar2=None, op0=ALU.add), reads=R, writes=R)
            P.dve(lambda e: e.reciprocal(out=scores, in_=sc0), reads=R, writes=R)
            P.dve(lambda e: e.tensor_tensor(out=biased, in0=scores, in1=rbias, op=ALU.add), reads=R + [Bw], writes=R)
            for gi in range(8):
                P.dve(lambda e, gi=gi: e.max(out=top8g[:, gi, :], in_=biased[:, gi * 32:(gi + 1) * 32]), reads=R, writes=R)
            P.dve(lambda e: e.tensor_tensor(out=grp, in0=top8g[:, :, 0], in1=top8g[:, :, 1], op=ALU.add), reads=R, writes=R)
            P.dve(lambda e: e.max(out=g8, in_=grp), reads=R, writes=R)
            P.dve(lambda e: e.tensor_scalar(out=gmask, in0=grp, scalar1=g8[:, 3:4], scalar2=None, op0=ALU.is_ge), reads=R, writes=R)
            P.dve(lambda e: e.scalar_tensor_tensor(out=mb.rearrange("p (g k) -> p g k", k=32), in0=biased.rearrange("p (g k) -> p g k", k=32),
                                                   scalar=1.0, in1=gmask.unsqueeze(2).to_broadcast([128, 8, 32]), op0=ALU.add, op1=ALU.mult),
                  reads=R, writes=R)
            P.dve(lambda e: e.max(out=v8, in_=mb), reads=R, writes=R)
            P.dve(lambda e: e.tensor_scalar(out=sel, in0=mb, scalar1=v8[:, 7:8], scalar2=None, op0=ALU.is_ge), reads=R, writes=R)
            P.dve(lambda e: e.scalar_tensor_tensor(out=wsel, in0=scores, scalar=1.0, in1=sel, op0=ALU.mult, op1=ALU.mult, accum_out=sumw[:, 0:1]),
                  reads=R, writes=R)
            P.dve(lambda e: e.reciprocal(out=sumw[:, 1:2], in_=sumw[:, 0:1]), reads=R, writes=R)
            P.dve(lambda e: e.max(out=g8, in_=wsel), reads=R, writes=R)
            P.dve(lambda e: e.max_index(out=i8, in_max=g8, in_values=wsel), reads=R, writes=R)
            P.dve(lambda e: e.tensor_scalar(out=gsel, in0=g8, scalar1=sumw[:, 1:2], scalar2=2.5, op0=ALU.mult, op1=ALU.mult), reads=R, writes=R)
            P.pe(lambda e: e.matmul(ps[6][:, 0:256], lhsT=lt_bf, rhs=sel, start=True, stop=True, skip_group_check=True),
                 reads=R + [Bcst], writes=[Bps[6]])
            P.pe(lambda e: e.matmul(ps[6][:, 256:512], lhsT=ones_bf, rhs=sel, start=True, stop=True, skip_group_check=True), reads=R + [Bcst], writes=[Bps[6]])
            P.dve(lambda e: e.tensor_tensor(out=pos, in0=ps[6][:, 0:256], in1=posbase, op=ALU.add), reads=[Bps[6], Bpb], writes=R)
            P.dve(lambda e: e.tensor_tensor(out=posbase, in0=ps[6][:, 256:512], in1=posbase, op=ALU.add), reads=[Bps[6], Bpb], writes=[Bpb])
            P.dve(lambda e: e.tensor_copy(out=i8f, in_=i8), reads=R, writes=R)
            for k in range(8):
                P.dve(lambda e, k=k: e.scalar_tensor_tensor(out=junk, in0=iota_f, scalar=i8f[:, k:k + 1], in1=pos, op0=ALU.is_equal, op1=ALU.mult,
                                                            accum_out=psel[:, k:k + 1]), reads=R + [Bcst], writes=R)
            P.dve(lambda e: e.tensor_scalar(out=tmpc, in0=psel, scalar1=float(CAP - 1), scalar2=None, op0=ALU.min), reads=R, writes=R)
            P.dve(lambda e: e.scalar_tensor_tensor(out=destf, in0=i8f, scalar=float(CAP), in1=tmpc, op0=ALU.mult, op1=ALU.add), reads=R, writes=R)
            P.dve(lambda e, g=g: e.tensor_copy(out=destall[:, g, :], in_=destf), reads=R, writes=[Bdest[g]])
            P.dve(lambda e: e.tensor_scalar(out=tfl, in0=tmpc, scalar1=128.0, scalar2=None, op0=ALU.is_ge), reads=R, writes=R)
            P.dve(lambda e: e.scalar_tensor_tensor(out=ppos, in0=tfl, scalar=-128.0, in1=tmpc, op0=ALU.mult, op1=ALU.add), reads=R, writes=R)
            P.dve(lambda e: e.scalar_tensor_tensor(out=tfl, in0=i8f, scalar=2.0, in1=tfl, op0=ALU.mult, op1=ALU.add), reads=R, writes=R)
            P.dve(lambda e: e.scalar_tensor_tensor(out=ppos, in0=ppos, scalar=512.0, in1=tfl, op0=ALU.mult, op1=ALU.add), reads=R, writes=R)
            P.dve(lambda e, pb=pb: e.tensor_copy(out=dslot[pb], in_=ppos), reads=R, writes=[Bds[pb]])
            P.dve(lambda e, g=g, pb=pb: e.tensor_copy(out=info[pb][:, :, 0], in_=tok_f[:, g:g + 1].to_broadcast([128, 8])),
                  reads=[Bcst], writes=[Binfo[pb]])
            P.dve(lambda e, pb=pb: e.tensor_copy(out=info[pb].bitcast(F32)[:, :, 1], in_=gsel), reads=R, writes=[Binfo[pb]])
            P.dve(lambda e, pb=pb: e.tensor_copy(out=info[pb][:, :, 2], in_=destf), reads=R, writes=[Binfo[pb]])
            for k in range(0 if NOSCAT else 8):
                P.dma("pool", lambda e, g=g, k=k, pb=pb: e.indirect_dma_start(
                    out=slot_s, out_offset=bass.IndirectOffsetOnAxis(ap=dslot[pb][:, k:k + 1], axis=0), in_=info[pb][:, k, :], in_offset=None),
                    reads=[Bds[pb], Binfo[pb], Bz])

        def cap(fn, g):
            P.cap_begin()
            if g < NT:
                fn(g)
            return P.cap_end()

        part1a(0)
        P.interleave(cap(part1b, 0), cap(part1a, 1))
        for g in range(NT):
            P.interleave(cap(part2b, g), cap(part2a, g), cap(part1b, g + 1), cap(part1a, g + 2))
        P.stage = 0
        if debug:
            P.dma("sp", lambda e: e.dma_start(out=dbg_dest, in_=destall.rearrange("p a b -> p (a b)")), reads=Bdest)
        P.barrier(bscr)
        ar.release(gmark)

    def phase_E():
        NW = 5
        wg = [ar.alloc([128, 8, 256], BF16) for _ in range(NW)]
        wu = [ar.alloc([128, 8, 256], BF16) for _ in range(NW)]
        wd = [ar.alloc([128, 2, 1024], BF16) for _ in range(NW)]
        siall = ar.alloc([128, NE * 2, 4], U32)
        xg = [ar.alloc([128, 1024], BF16) for _ in range(8)]
        xgT = [ar.alloc([128, 8, 256], BF16) for _ in range(2)]
        sl = [ar.alloc([128, 512], BF16) for _ in range(2)]
        aT = [ar.alloc([128, 2, 256], BF16) for _ in range(2)]
        yst = [ar.alloc([128, 1024], BF16) for _ in range(4)]
        Bwg = P.bufs(NW); Bwu = P.bufs(NW); Bwd = P.bufs(NW); Bsi = P.buf(); Bxg = P.bufs(8); BxgT = P.bufs(2)
        Bsl = P.bufs(2); BaT = P.bufs(2); Byst = P.bufs(4)
        si_f = siall.bitcast(F32)
        P.dma("sp", lambda e: e.dma_start(out=siall.rearrange("p r w -> p (r w)"), in_=slot_s.rearrange("(p r) w -> p (r w)", p=128)), writes=[Bsi])

        for i in range(8):
            P.dve(lambda e, i=i: e.memset(xg[i], 0.0), writes=[Bxg[i]])

        def W(ex):
            wb = ex % NW
            P.dma("pool", lambda e, ex=ex, wb=wb: e.dma_start(out=wg[wb].rearrange("p c f -> p (c f)"),
                                                             in_=w_exp_gate[ex].rearrange("(p c) f -> p (c f)", c=8)), writes=[Bwg[wb]])
            P.dma("pool", lambda e, ex=ex, wb=wb: e.dma_start(out=wu[wb].rearrange("p c f -> p (c f)"),
                                                             in_=w_exp_up[ex].rearrange("(p c) f -> p (c f)", c=8)), writes=[Bwu[wb]])
            P.dma("pool", lambda e, ex=ex, wb=wb: e.dma_start(out=wd[wb].rearrange("p c d -> p (c d)"),
                                                             in_=w_exp_down[ex].rearrange("(p c) d -> p (c d)", c=2)), writes=[Bwd[wb]])

        def Gt(ex):
            for t in range(2):
                xi = 2 * (ex % 4) + t
                P.dma("pool", lambda e, xi=xi, ex=ex, t=t: e.indirect_dma_start(
                    out=xg[xi], out_offset=None, in_=h2b_s, in_offset=bass.IndirectOffsetOnAxis(ap=siall[:, 2 * ex + t, 0:1], axis=0),
                    bounds_check=_breg(e, S - 1), oob_is_err=False), reads=[Bsi], writes=[Bxg[xi]])

        def T(ex):
            xb = ex % 2
            for t in range(2):
                xi = 2 * (ex % 4) + t
                for c in range(8):
                    P.pe(lambda e, c=c, xi=xi, t=t: e.transpose(out=psb(t)[:, c * 128:(c + 1) * 128], in_=xg[xi][:, c::8],
                                                                identity=ident_bf), reads=[Bxg[xi], Bcst], writes=[Bps[t]])
                src_v = psb(t)[:, :].rearrange("p (a b) -> p a b", a=8)
                if t == 0:
                    P.act(lambda e, xb=xb, t=t, src_v=src_v: e.activation(out=xgT[xb][:, :, t * 128:(t + 1) * 128], in_=src_v, func=AF.Copy),
                          reads=[Bps[t]], writes=[BxgT[xb]])
                else:
                    P.dve(lambda e, xb=xb, t=t, src_v=src_v: e.tensor_copy(out=xgT[xb][:, :, t * 128:(t + 1) * 128], in_=src_v),
                          reads=[Bps[t]], writes=[BxgT[xb]])

        def H(ex):
            wb = ex % NW
            xb = ex % 2
            for fc in range(4):
                bank = 2 + fc // 2
                w_, Bw_ = (wg, Bwg) if fc < 2 else (wu, Bwu)
                f0 = (fc % 2) * 128
                for c in range(8):
                    P.pe(lambda e, c=c, bank=bank, w_=w_, f0=f0, fc=fc, wb=wb, xb=xb: e.matmul(
                        ps[bank][:, (fc % 2) * 256:(fc % 2 + 1) * 256], lhsT=w_[wb][:, c, (fc % 2)::2], rhs=xgT[xb][:, c, :],
                        start=(c == 0), stop=(c == 7), skip_group_check=True), reads=[Bw_[wb], BxgT[xb]], writes=[Bps[bank]])
            P.act(lambda e, xb=xb: e.activation(out=sl[xb], in_=ps[2][:, :], func=AF.Silu), reads=[Bps[2]], writes=[Bsl[xb]])
            P.dve(lambda e, xb=xb: e.tensor_tensor(out=aT[xb].rearrange("p a b -> p (a b)"), in0=sl[xb], in1=ps[3][:, :], op=ALU.mult),
                  reads=[Bsl[xb], Bps[3]], writes=[BaT[xb]])

        def Y(ex):
            wb = ex % NW
            xb = ex % 2
            for t in range(2):
                yi = 2 * xb + t
                gate_ap = si_f[:, 2 * ex + t, 1:2]
                for hf in range(2):
                    bank = 4 + 2 * t + hf
                    for f2 in range(2):
                        P.pe(lambda e, bank=bank, f2=f2, t=t, hf=hf, xb=xb, wb=wb: e.matmul(
                            ps[bank][:, :], lhsT=aT[xb][:, f2, t * 128:(t + 1) * 128], rhs=wd[wb][:, f2, hf * 512:(hf + 1) * 512],
                            start=(f2 == 0), stop=(f2 == 1)), reads=[BaT[xb], Bwd[wb]], writes=[Bps[bank]])
                    if hf == 0:
                        P.act(lambda e, bank=bank, yi=yi, gate_ap=gate_ap: e.activation(out=yst[yi][:, 0:512], in_=ps[bank][:, :], func=AF.Copy,
                                                                                        scale=gate_ap), reads=[Bps[bank], Bsi], writes=[Byst[yi]])
                    else:
                        P.dve(lambda e, bank=bank, yi=yi, gate_ap=gate_ap: e.tensor_scalar(out=yst[yi][:, 512:1024], in0=ps[bank][:, :],
                                                                                           scalar1=gate_ap, scalar2=None, op0=ALU.mult),
                              reads=[Bps[bank], Bsi], writes=[Byst[yi]])
                P.dma("pool", lambda e, yi=yi, ex=ex, t=t: e.indirect_dma_start(
                    out=Y_s, out_offset=bass.IndirectOffsetOnAxis(ap=siall[:, 2 * ex + t, 2:3], axis=0), in_=yst[yi], in_offset=None,
                    bounds_check=_breg(e, NE * CAP - 1), oob_is_err=False), reads=[Byst[yi], Bsi])

        W(0); W(1); W(2); Gt(0); Gt(1); Gt(2)
        T(0)
        for ex in range(NE):
            if ex + 3 < NE:
                W(ex + 3)
                Gt(ex + 3)
            H(ex)
            if ex + 1 < NE:
                T(ex + 1)
            Y(ex)
        P.barrier(bscr)
        ar.release(gmark)

    def phase_F():
        g2b = ar.alloc([128, 1024], F32); b2b = ar.alloc([128, 1024], F32)
        NB = 4
        h2t = [ar.alloc([128, 1024], F32) for _ in range(NB)]
        ysht = [ar.alloc([128, 1024], F32) for _ in range(NB)]
        Yk = [[ar.alloc([128, 1024], BF16) for _ in range(8)] for _ in range(NB)]
        stt = [ar.alloc([128, 2, 6], F32) for _ in range(NB)]
        mv = [ar.alloc([128, 4], F32) for _ in range(NB)]
        Bw = P.buf(); Bh = P.bufs(NB); Bys = P.bufs(NB); BY = [P.bufs(8) for _ in range(NB)]; Bmv = P.bufs(NB)
        bcast_load(g2b, ln2_g, Bw); bcast_load(b2b, ln2_b, Bw)
        def tile_F(g):
            pb = g % NB
            rows = slice(g * 128, (g + 1) * 128)
            bk = 2 * (g % 4)
            P.dma("pool", lambda e, pb=pb, rows=rows: e.dma_start(out=h2t[pb], in_=h2_s[rows, :]), writes=[Bh[pb]])
            P.dma("pool", lambda e, pb=pb, rows=rows: e.dma_start(out=ysht[pb], in_=ysh_s[rows, :]), writes=[Bys[pb]])
            for k in range(8):
                P.dma("pool", lambda e, pb=pb, k=k, g=g: e.indirect_dma_start(
                    out=Yk[pb][k], out_offset=None, in_=Y_s, in_offset=bass.IndirectOffsetOnAxis(ap=destall[:, g, k:k + 1], axis=0)),
                    reads=[Bdest[g]], writes=[BY[pb][k]])
            for hf in range(2):
                for k in range(8):
                    P.pe(lambda e, pb=pb, k=k, hf=hf, bk=bk: e.matmul(ps[bk + hf][:, :], lhsT=ident_bf, rhs=Yk[pb][k][:, hf * 512:(hf + 1) * 512],
                                                                     start=(k == 0), stop=(k == 7)), reads=[BY[pb][k], Bcst], writes=[Bps[bk + hf]])
            r = h2t[pb]
            P.dve(lambda e, r=r, pb=pb: e.scalar_tensor_tensor(out=r, in0=r, scalar=ALPHA, in1=ysht[pb], op0=ALU.mult, op1=ALU.add),
                  reads=[Bh[pb], Bys[pb]], writes=[Bh[pb]])
            for hf in range(2):
                P.dve(lambda e, r=r, hf=hf, bk=bk: e.tensor_tensor(out=r[:, hf * 512:(hf + 1) * 512], in0=r[:, hf * 512:(hf + 1) * 512],
                                                                  in1=ps[bk + hf][:, :], op=ALU.add), reads=[Bh[pb], Bps[bk + hf]], writes=[Bh[pb]])
            ln_stats(r, mv[pb], stt[pb], mv[pb][:, 2:3], Bh[pb], Bmv[pb])
            P.dve(lambda e, pb=pb: e.scalar_tensor_tensor(out=mv[pb][:, 3:4], in0=mv[pb][:, 0:1], scalar=-1.0, in1=mv[pb][:, 2:3],
                                                          op0=ALU.mult, op1=ALU.mult), reads=[Bmv[pb]], writes=[Bmv[pb]])
            P.act(lambda e, r=r, pb=pb: e.activation(out=r, in_=r, func=AF.Identity, scale=mv[pb][:, 2:3], bias=mv[pb][:, 3:4]),
                  reads=[Bh[pb], Bmv[pb]], writes=[Bh[pb]])
            P.dve(lambda e, r=r: e.tensor_tensor(out=r, in0=r, in1=g2b, op=ALU.mult), reads=[Bh[pb], Bw], writes=[Bh[pb]])
            P.dve(lambda e, r=r: e.tensor_tensor(out=r, in0=r, in1=b2b, op=ALU.add), reads=[Bh[pb], Bw], writes=[Bh[pb]])
            def store_fn(r=r, rows=rows):
                fn_ = lambda e: e.dma_start(out=out[rows, :], in_=r)
                fn_.is_out = True
                return fn_
            P.dma("sp", store_fn(), reads=[Bh[pb]])

        for g2 in range(0, NT, 2):
            P.cap_begin(); tile_F(g2); la = P.cap_end()
            P.cap_begin(); tile_F(g2 + 1); lb = P.cap_end()
            P.interleave(la, lb)
        outs.extend(o for o in P.ops if o.dma and getattr(o.fn, "is_out", False))

    phase_A()
    if stop_after >= "B":
        phase_B()
    if stop_after >= "C":
        phase_C()
    if stop_after >= "D":
        phase_D()
    if stop_after >= "E":
        phase_E()
    if stop_after >= "F":
        phase_F()
    P.emit(final_wait_ops=outs)
    return nc, P


def _consts():
    c = np.zeros((128, C_END), np.float32)
    p = np.arange(128)[:, None]
    j = np.arange(128)[None, :]
    c[:, C_ID:C_ID + 128] = (p == j)
    c[:, C_LE:C_LE + 128] = (p <= j)
    c[:, C_LT:C_LT + 128] = (p < j)
    c[:, C_IOTA:C_IOTA + 256] = np.arange(256)[None, :]
    c[:, C_TOK:C_TOK + 32] = np.arange(32)[None, :] * 128 + p
    return c


def _slot_init():
    row = np.array([S + 4095, 0, NE * CAP + 4095, 0], np.uint32)
    return np.ascontiguousarray(np.tile(row, (128, 512)))


def make_in_map(inputs, b):
    g = np.asarray(inputs["ln_in_g"], np.float32)
    bb = np.asarray(inputs["ln_in_b"], np.float32)
    lnfm = np.concatenate([g.reshape(8, 128).T, bb.reshape(8, 128).T], axis=1)
    m = {"x": np.ascontiguousarray(inputs["x"][b]), "cst": _consts(), "slot_init": _slot_init(), "lnfm": np.ascontiguousarray(lnfm),
         "ln_in_g": g, "ln_in_b": bb}
    for k in IN_NAMES:
        if k in m:
            continue
        m[k] = np.ascontiguousarray(np.asarray(inputs[k])[0])
    return m


_CACHE = {}


def kernel(**inputs):
    if "nc" not in _CACHE:
        _CACHE["nc"] = build()[0]
    nc = _CACHE["nc"]
    in_maps = [make_in_map(inputs, b) for b in range(8)]
    res = run_bass_kernel_spmd(nc, in_maps, core_ids=list(range(8)))
    return np.stack([np.asarray(r["out"], np.float32) for r in res.results], axis=0)
```
